# Optimizing a Trainium2 kernel written in Bass

```python
import jax
import jax.numpy as jnp
from jax import lax
import numpy as np

D_MODEL = 1024
BATCH = 8
SEQ = 4096
DEPTH = 4

GRID_W = 64
CTX_LEN = 256
EPS = 1e-6
ROPE_THETA = 10000.0
NEG_BIG = -1e30
F_FLOOR = 1e-20

HEAD_DIM = D_MODEL // 16
GROUP_HEADS = 4
GROUP_W = GROUP_HEADS * HEAD_DIM
D_MIX = 4 * GROUP_W

MLA_H = GROUP_HEADS
MLA_Q_LORA = 3 * D_MODEL // 16
MLA_KV_LORA = D_MODEL // 8
MLA_NOPE = HEAD_DIM
MLA_ROPE = HEAD_DIM // 2
MLA_V = HEAD_DIM
MLA_SCALE = (MLA_NOPE + MLA_ROPE) ** -0.5
ATTN_BLOCK = 128
NA_H = GROUP_HEADS
NA_KH_MAX = 8
NA_KW = 16
NA_SCALE = HEAD_DIM ** -0.5
RET_H = GROUP_HEADS
RET_DK = HEAD_DIM
RET_DV = HEAD_DIM
RET_CHUNK = 128
HG_H = GROUP_HEADS
HG_DK = HEAD_DIM
HG_DV = HEAD_DIM
HG_CHUNK = 16
MOE_GROUPS = 4
MOE_PER_GROUP = 8
MOE_EXPERTS = MOE_GROUPS * MOE_PER_GROUP
MOE_TOPK = 2
MOE_FF = D_MODEL // 2
MOE_BLOCK = 256

IN_SPLITS = (MLA_Q_LORA, MLA_KV_LORA, MLA_ROPE,
             NA_H * HEAD_DIM, NA_H * HEAD_DIM, NA_H * HEAD_DIM,
             RET_H * RET_DK, RET_H * RET_DK, RET_H * RET_DV, RET_H * RET_DV,
             HG_H * HG_DK, HG_H * HG_DK, HG_H * HG_DK, HG_H * HG_DV, HG_H * HG_DV)
P_TOT = sum(IN_SPLITS)

kernel_name = 'hybrid_mla_na_retnet_hgrn2_hmoe_dit'


def rms_norm(x, gain=None):
    x32 = x.astype(jnp.float32)
    y = x32 * lax.rsqrt(jnp.mean(jnp.square(x32), axis=-1, keepdims=True) + EPS)
    if gain is not None:
        y = y * gain.astype(jnp.float32)
    return y.astype(x.dtype)


def modulate(x, shift, scale):
    return rms_norm(x) * (1 + scale) + shift


def split_cols(p):
    offsets = [int(o) for o in np.cumsum(IN_SPLITS)[:-1]]
    return jnp.split(p, offsets, axis=-1)


def split_heads(t, n_heads):
    b, n, _ = t.shape
    return t.reshape(b, n, n_heads, -1).transpose(0, 2, 1, 3)


def flip_t(t):
    return jnp.flip(t, axis=2)


def gated_head_norm(o, g, gain):
    b, h, n, dv = o.shape
    return rms_norm(o.transpose(0, 2, 1, 3), gain).reshape(b, n, h * dv) * jax.nn.silu(g)


def axial_rope_tables(n_tokens):
    pos = jnp.arange(n_tokens)
    rows = (pos // GRID_W).astype(jnp.float32)
    cols = (pos % GRID_W).astype(jnp.float32)
    per_axis = MLA_ROPE // 2
    inv_freq = ROPE_THETA ** (-jnp.arange(0, per_axis, 2, dtype=jnp.float32) / per_axis)
    ang = jnp.concatenate([rows[:, None] * inv_freq, cols[:, None] * inv_freq], axis=-1)
    return jnp.cos(ang), jnp.sin(ang)


def _rotate_pairs(z, cos, sin):
    m = z.shape[-1] // 2
    z1, z2 = z[..., :m], z[..., m:]
    return jnp.concatenate([z1 * cos - z2 * sin, z2 * cos + z1 * sin], axis=-1)


def apply_axial_rope(x, cos, sin):
    half = x.shape[-1] // 2
    m = half // 2
    cos = cos.astype(x.dtype)
    sin = sin.astype(x.dtype)
    return jnp.concatenate([_rotate_pairs(x[..., :half], cos[..., :m], sin[..., :m]),
                            _rotate_pairs(x[..., half:], cos[..., m:], sin[..., m:])], axis=-1)


def mla_queries(cq, g_cq, w_uq, g_qn, g_qr):
    b, n, _ = cq.shape
    q = (rms_norm(cq, g_cq) @ w_uq).reshape(b, n, MLA_H, MLA_NOPE + MLA_ROPE)
    return rms_norm(q[..., :MLA_NOPE], g_qn), rms_norm(q[..., MLA_NOPE:], g_qr)


def mla_keys(ckv, kr, g_ckv, w_ukv, g_kn, g_kr):
    b, n, _ = ckv.shape
    kv = (rms_norm(ckv, g_ckv) @ w_ukv).reshape(b, n, MLA_H, MLA_NOPE + MLA_V)
    return rms_norm(kv[..., :MLA_NOPE], g_kn), rms_norm(kr, g_kr), kv[..., MLA_NOPE:]


def mla_scores(q_nope, q_rope, k_nope, k_rope):
    s = jnp.einsum('bqhd,bkhd->bhqk', q_nope, k_nope) + jnp.einsum('bqhr,bkr->bhqk', q_rope, k_rope)
    return s.astype(jnp.float32) * MLA_SCALE


def mla_mixer(lat, ctx, cos, sin, q_prm, k_prm, with_ctx_out):
    cq, ckv, kr = lat
    cq_c, ckv_c, kr_c = ctx
    qn, qr = mla_queries(cq, *q_prm)
    kn, kr, v = mla_keys(ckv, kr, *k_prm)
    kn_c, kr_c, v_c = mla_keys(ckv_c, kr_c, *k_prm)
    qr = apply_axial_rope(qr, cos[:, None, :], sin[:, None, :])
    kr = apply_axial_rope(kr, cos, sin)
    b, n = qn.shape[:2]
    nb = n // ATTN_BLOCK

    def to_blocks(t):
        return jnp.moveaxis(t.reshape(b, nb, ATTN_BLOCK, *t.shape[2:]), 1, 0)

    def attend_block(blk):
        qn_b, qr_b = blk
        s = jnp.concatenate([mla_scores(qn_b, qr_b, kn, kr), mla_scores(qn_b, qr_b, kn_c, kr_c)], axis=-1)
        p = jax.nn.softmax(s, axis=-1).astype(v.dtype)
        return (jnp.einsum('bhqk,bkhd->bqhd', p[..., :n], v)
                + jnp.einsum('bhqk,bkhd->bqhd', p[..., n:], v_c))

    o = lax.map(attend_block, (to_blocks(qn), to_blocks(qr)))
    y = jnp.moveaxis(o, 0, 1).reshape(b, n, MLA_H * MLA_V)
    if not with_ctx_out:
        return y, None
    qn_c, qr_c = mla_queries(cq_c, *q_prm)
    p_c = jax.nn.softmax(mla_scores(qn_c, qr_c, kn_c, kr_c), axis=-1).astype(v_c.dtype)
    y_c = jnp.einsum('bhqk,bkhd->bqhd', p_c, v_c).reshape(b, v_c.shape[1], MLA_H * MLA_V)
    return y, y_c


def na_mixer(lat, ctx, rows, g_q, g_k, rpb, with_ctx_out):
    q, k, v = lat
    qc, kc, vc = ctx
    b, n, _ = q.shape
    kh = min(NA_KH_MAX, rows)
    kw = min(NA_KW, GRID_W)

    def heads(t):
        return t.reshape(t.shape[0], t.shape[1], NA_H, HEAD_DIM)

    qg = rms_norm(heads(q), g_q).reshape(b, rows, GRID_W, NA_H, HEAD_DIM)
    kg = rms_norm(heads(k), g_k).reshape(b, rows, GRID_W, NA_H, HEAD_DIM)
    vg = heads(v).reshape(b, rows, GRID_W, NA_H, HEAD_DIM)
    k_c = rms_norm(heads(kc), g_k)
    v_c = heads(vc)
    r = jnp.arange(rows)
    w = jnp.arange(GRID_W)
    row_idx = jnp.clip(r - kh // 2, 0, rows - kh)[:, None] + jnp.arange(kh)[None, :]
    col_start = jnp.clip(w - kw // 2, 0, GRID_W - kw)
    col_valid = (w[None, :] >= col_start[:, None]) & (w[None, :] < col_start[:, None] + kw)
    k_band = kg[:, row_idx]
    v_band = vg[:, row_idx]
    dr = row_idx - r[:, None] + (NA_KH_MAX - 1)
    dc = jnp.clip(w[None, :] - w[:, None], 1 - kw, kw - 1) + (NA_KW - 1)
    bias = rpb[:, dr[:, None, :, None], dc[None, :, None, :]].astype(jnp.float32)
    s_win = jnp.einsum('brwhd,brkuhd->bhrwku', qg, k_band).astype(jnp.float32) * NA_SCALE + bias
    s_win = jnp.where(col_valid[:, None, :], s_win, NEG_BIG)
    s_ctx = jnp.einsum('brwhd,bchd->bhrwc', qg, k_c).astype(jnp.float32) * NA_SCALE
    n_win = kh * GRID_W
    s = jnp.concatenate([s_win.reshape(b, NA_H, rows, GRID_W, n_win), s_ctx], axis=-1)
    p = jax.nn.softmax(s, axis=-1).astype(vg.dtype)
    p_win = p[..., :n_win].reshape(b, NA_H, rows, GRID_W, kh, GRID_W)
    o = (jnp.einsum('bhrwku,brkuhd->brwhd', p_win, v_band)
         + jnp.einsum('bhrwc,bchd->brwhd', p[..., n_win:], v_c))
    y = o.reshape(b, n, NA_H * HEAD_DIM)
    if not with_ctx_out:
        return y, None
    q_c = rms_norm(heads(qc), g_q)
    p_c = jax.nn.softmax(jnp.einsum('bqhd,bkhd->bhqk', q_c, k_c).astype(jnp.float32) * NA_SCALE, axis=-1)
    y_c = jnp.einsum('bhqk,bkhd->bqhd', p_c.astype(v_c.dtype), v_c).reshape(b, qc.shape[1], NA_H * HEAD_DIM)
    return y, y_c


def retention_log_decays():
    j = jnp.arange(2 * RET_H, dtype=jnp.float32)
    lg = jnp.log1p(-jnp.exp2(-5.0 - j))
    return lg[0::2], lg[1::2]


def retention_scan(q, k, v, log_gamma, s0):
    b, h, n, dk = k.shape
    dv = v.shape[-1]
    dt = k.dtype
    cs = min(RET_CHUNK, n)
    nc = n // cs
    kc = k.reshape(b, h, nc, cs, dk)
    vc = v.reshape(b, h, nc, cs, dv)
    pos = jnp.arange(cs, dtype=jnp.float32)
    lg = log_gamma[:, None]
    k_w = jnp.exp((cs - 1 - pos)[None] * lg).astype(dt)
    d_state = jnp.einsum('bhncd,bhnce->bhnde', kc * k_w[None, :, None, :, None], vc)
    chunk_decay = jnp.exp(cs * log_gamma).astype(dt)[None, :, None, None]

    def step(s, ds):
        return chunk_decay * s + ds, s

    s_final, s_prev = lax.scan(step, s0, jnp.moveaxis(d_state, 2, 0))
    if q is None:
        return None, s_final
    qc = q.reshape(b, h, nc, cs, dk)
    diff = pos[:, None] - pos[None, :]
    decay = jnp.where(diff >= 0, jnp.exp(jnp.maximum(diff, 0.0)[None] * log_gamma[:, None, None]), 0.0).astype(dt)
    q_w = jnp.exp((pos + 1)[None] * lg).astype(dt)
    scores = jnp.einsum('bhntd,bhnsd->bhnts', qc, kc) * decay[None, :, None]
    o = (jnp.einsum('bhnts,bhnse->bhnte', scores, vc)
         + jnp.einsum('bhntd,bhnde->bhnte', qc * q_w[None, :, None, :, None], jnp.moveaxis(s_prev, 0, 2)))
    return o.reshape(b, h, n, dv), s_final


def retention_mixer(lat, ctx, g_out, with_ctx_out):
    q, k, v, g = lat
    qc, kc, vc, gc = ctx
    lg_f, lg_b = retention_log_decays()
    k_scale = RET_DK ** -0.5
    qh, kh, vh = split_heads(q, RET_H), split_heads(k, RET_H) * k_scale, split_heads(v, RET_H)
    qch = split_heads(qc, RET_H) if with_ctx_out else None
    kch, vch = split_heads(kc, RET_H) * k_scale, split_heads(vc, RET_H)
    zero = jnp.zeros((q.shape[0], RET_H, RET_DK, RET_DV), k.dtype)
    oc_f, s_f = retention_scan(qch, kch, vch, lg_f, zero)
    oc_b, s_b = retention_scan(None if qch is None else flip_t(qch), flip_t(kch), flip_t(vch), lg_b, zero)
    o_f, _ = retention_scan(qh, kh, vh, lg_f, s_f)
    o_b, _ = retention_scan(flip_t(qh), flip_t(kh), flip_t(vh), lg_b, s_b)
    y = gated_head_norm(o_f + flip_t(o_b), g, g_out)
    if not with_ctx_out:
        return y, None
    return y, gated_head_norm(oc_f + flip_t(oc_b), gc, g_out)


def hgrn2_log_forget(f_raw, lb):
    lb = lb.astype(jnp.float32)
    f = lb + (1.0 - lb) * jax.nn.sigmoid(f_raw.astype(jnp.float32))
    return split_heads(jnp.log(jnp.maximum(f, F_FLOOR)), HG_H)


def chunk_gla(q, k, v, log_f, s0):
    b, h, n, dk = k.shape
    dv = v.shape[-1]
    dt = k.dtype
    cs = min(HG_CHUNK, n)
    nc = n // cs
    kc = k.reshape(b, h, nc, cs, dk)
    vc = v.reshape(b, h, nc, cs, dv)
    a = jnp.cumsum(log_f.reshape(b, h, nc, cs, dk), axis=3)
    a_last = a[:, :, :, -1]
    k_state = kc * jnp.exp(a_last[:, :, :, None] - a).astype(dt)
    d_state = jnp.einsum('bhnsd,bhnse->bhnde', k_state, vc)
    chunk_decay = jnp.moveaxis(jnp.exp(a_last).astype(dt), 2, 0)[..., None]

    def step(s, inp):
        dec, ds = inp
        return dec * s + ds, s

    s_final, s_prev = lax.scan(step, s0, (chunk_decay, jnp.moveaxis(d_state, 2, 0)))
    if q is None:
        return None, s_final
    qc = q.reshape(b, h, nc, cs, dk)
    tri = jnp.tril(jnp.ones((cs, cs), bool))[:, :, None]
    rel = a[:, :, :, :, None, :] - a[:, :, :, None, :, :]
    decay = jnp.where(tri, jnp.exp(jnp.where(tri, rel, 0.0)), 0.0).astype(dt)
    scores = jnp.einsum('bhntd,bhnsd,bhntsd->bhnts', qc, kc, decay)
    o = (jnp.einsum('bhnts,bhnse->bhnte', scores, vc)
         + jnp.einsum('bhntd,bhnde->bhnte', qc * jnp.exp(a).astype(dt), jnp.moveaxis(s_prev, 0, 2)))
    return o.reshape(b, h, n, dv), s_final


def hgrn2_mixer(lat, ctx, lb, g_out, with_ctx_out):
    q, f_fw, f_bw, i, g = lat
    qc, fc_fw, fc_bw, ic, gc = ctx
    dt = q.dtype

    def gates(f_raw):
        lf = hgrn2_log_forget(f_raw, lb)
        return (-jnp.expm1(lf)).astype(dt), lf

    qh, vh = split_heads(jax.nn.silu(q), HG_H), split_heads(i, HG_H)
    k_f, lf_f = gates(f_fw)
    k_b, lf_b = gates(f_bw)
    qch = split_heads(jax.nn.silu(qc), HG_H) if with_ctx_out else None
    vch = split_heads(ic, HG_H)
    kc_f, lfc_f = gates(fc_fw)
    kc_b, lfc_b = gates(fc_bw)
    zero = jnp.zeros((q.shape[0], HG_H, HG_DK, HG_DV), dt)
    oc_f, s_f = chunk_gla(qch, kc_f, vch, lfc_f, zero)
    oc_b, s_b = chunk_gla(None if qch is None else flip_t(qch), flip_t(kc_b), flip_t(vch), flip_t(lfc_b), zero)
    o_f, _ = chunk_gla(qh, k_f, vh, lf_f, s_f)
    o_b, _ = chunk_gla(flip_t(qh), flip_t(k_b), flip_t(vh), flip_t(lf_b), s_b)
    y = gated_head_norm(o_f + flip_t(o_b), g, g_out)
    if not with_ctx_out:
        return y, None
    return y, gated_head_norm(oc_f + flip_t(oc_b), gc, g_out)


def hier_moe(h, w_rg, b_rg, w_re, b_re, w1, w3, w2):
    n, d = h.shape
    p_group = jax.nn.softmax((h @ w_rg + b_rg).astype(jnp.float32), axis=-1)
    pg_top, g_idx = lax.top_k(p_group, 1)
    fine = (h @ w_re + b_re).astype(jnp.float32).reshape(n, MOE_GROUPS, MOE_PER_GROUP)
    fine = fine[jnp.arange(n), g_idx[:, 0]]
    pe_top, e_local = lax.top_k(jax.nn.softmax(fine, axis=-1), MOE_TOPK)
    gate = pg_top * pe_top / jnp.sum(pe_top, axis=-1, keepdims=True)
    expert = g_idx * MOE_PER_GROUP + e_local

    n_assign = n * MOE_TOPK
    flat_e = expert.reshape(-1)
    flat_tok = jnp.repeat(jnp.arange(n, dtype=jnp.int32), MOE_TOPK)
    flat_gate = gate.reshape(-1).astype(h.dtype)
    order = jnp.argsort(flat_e)
    e_sorted = flat_e[order]
    counts = jnp.zeros((MOE_EXPERTS,), jnp.int32).at[flat_e].add(1)
    start = jnp.cumsum(counts) - counts
    padded = (counts + MOE_BLOCK - 1) // MOE_BLOCK * MOE_BLOCK
    pad_end = jnp.cumsum(padded)
    pad_start = pad_end - padded
    dest = pad_start[e_sorted] + jnp.arange(n_assign, dtype=jnp.int32) - start[e_sorted]
    n_blocks = -(-n_assign // MOE_BLOCK) + MOE_EXPERTS
    n_rows = n_blocks * MOE_BLOCK
    row_tok = jnp.full((n_rows,), n, jnp.int32).at[dest].set(flat_tok[order])
    row_gate = jnp.zeros((n_rows,), h.dtype).at[dest].set(flat_gate[order])
    block_expert = jnp.minimum(
        jnp.searchsorted(pad_end, jnp.arange(n_blocks, dtype=jnp.int32) * MOE_BLOCK, side='right'),
        MOE_EXPERTS - 1)
    h_pad = jnp.concatenate([h, jnp.zeros((1, d), h.dtype)], axis=0)
    x_rows = h_pad[row_tok].reshape(n_blocks, MOE_BLOCK, d)

    def expert_block(args):
        xb, e = args
        return (jax.nn.silu(xb @ w1[e]) * (xb @ w3[e])) @ w2[e]

    y_rows = lax.map(expert_block, (x_rows, block_expert)).reshape(n_rows, d) * row_gate[:, None]
    return jnp.zeros((n + 1, d), h.dtype).at[row_tok].add(y_rows)[:n]


def setup_inputs(seed: int = 0) -> dict:
    key = jax.random.key(seed)
    ks = iter(jax.random.split(key, 32))
    f32 = jnp.float32
    L, D = DEPTH, D_MODEL

    def nrm(shape, scale):
        return jax.random.normal(next(ks), shape, f32) * scale

    def gain(shape):
        return 1.0 + nrm(shape, 0.02)

    return {
        'x': nrm((BATCH, SEQ, D), 1.0),
        'c': nrm((BATCH, D), 1.0),
        'ctx': nrm((BATCH, CTX_LEN, D), 1.0),
        'c_ctx': nrm((D,), 1.0),
        'w_ada': nrm((L, D, 6 * D), 0.5 * D ** -0.5),
        'b_ada': nrm((L, 6 * D), 0.01),
        'w_in': nrm((L, D, P_TOT), D ** -0.5),
        'w_out': nrm((L, D_MIX, D), D_MIX ** -0.5),
        'mla_g_cq': gain((L, MLA_Q_LORA)),
        'mla_g_ckv': gain((L, MLA_KV_LORA)),
        'mla_w_uq': nrm((L, MLA_Q_LORA, MLA_H * (MLA_NOPE + MLA_ROPE)), MLA_Q_LORA ** -0.5),
        'mla_w_ukv': nrm((L, MLA_KV_LORA, MLA_H * (MLA_NOPE + MLA_V)), MLA_KV_LORA ** -0.5),
        'mla_g_qn': gain((L, MLA_NOPE)),
        'mla_g_qr': gain((L, MLA_ROPE)),
        'mla_g_kn': gain((L, MLA_NOPE)),
        'mla_g_kr': gain((L, MLA_ROPE)),
        'na_g_q': gain((L, HEAD_DIM)),
        'na_g_k': gain((L, HEAD_DIM)),
        'na_rpb': nrm((L, NA_H, 2 * NA_KH_MAX - 1, 2 * NA_KW - 1), 0.1),
        'ret_g_out': gain((L, RET_DV)),
        'hg_lb_raw': nrm((L, HG_H * HG_DK), 1.0),
        'hg_g_out': gain((L, HG_DV)),
        'moe_w_rg': nrm((L, D, MOE_GROUPS), D ** -0.5),
        'moe_b_rg': nrm((L, MOE_GROUPS), 0.01),
        'moe_w_re': nrm((L, D, MOE_EXPERTS), D ** -0.5),
        'moe_b_re': nrm((L, MOE_EXPERTS), 0.01),
        'moe_w1': nrm((L, MOE_EXPERTS, D, MOE_FF), D ** -0.5),
        'moe_w3': nrm((L, MOE_EXPERTS, D, MOE_FF), D ** -0.5),
        'moe_w2': nrm((L, MOE_EXPERTS, MOE_FF, D), MOE_FF ** -0.5),
    }


def reference(x, c, ctx, c_ctx, w_ada, b_ada, w_in, w_out, mla_g_cq, mla_g_ckv, mla_w_uq, mla_w_ukv,
              mla_g_qn, mla_g_qr, mla_g_kn, mla_g_kr, na_g_q, na_g_k, na_rpb, ret_g_out, hg_lb_raw,
              hg_g_out, moe_w_rg, moe_b_rg, moe_w_re, moe_b_re, moe_w1, moe_w3, moe_w2):
    b, n, d = x.shape
    n_ctx = ctx.shape[1]
    rows = n // GRID_W
    cos, sin = axial_rope_tables(n)
    lb_w = jax.nn.softmax(hg_lb_raw.astype(jnp.float32), axis=0)
    hg_lb = jnp.cumsum(lb_w, axis=0) - lb_w[0:1]
    silu_c = jax.nn.silu(c)
    silu_cc = jax.nn.silu(c_ctx)
    xc = ctx
    for l in range(DEPTH):
        last = l == DEPTH - 1
        mod = silu_c @ w_ada[l] + b_ada[l]
        mod_c = silu_cc @ w_ada[l] + b_ada[l]
        sh_a, sc_a, gt_a, sh_m, sc_m, gt_m = jnp.split(mod[:, None, :], 6, axis=-1)
        csh_a, csc_a, cgt_a, csh_m, csc_m, cgt_m = jnp.split(mod_c, 6)

        p = split_cols(modulate(x, sh_a, sc_a) @ w_in[l])
        pc = split_cols(modulate(xc, csh_a, csc_a) @ w_in[l])
        y_mla, yc_mla = mla_mixer(p[0:3], pc[0:3], cos, sin,
                                  (mla_g_cq[l], mla_w_uq[l], mla_g_qn[l], mla_g_qr[l]),
                                  (mla_g_ckv[l], mla_w_ukv[l], mla_g_kn[l], mla_g_kr[l]), not last)
        y_na, yc_na = na_mixer(p[3:6], pc[3:6], rows, na_g_q[l], na_g_k[l], na_rpb[l], not last)
        y_ret, yc_ret = retention_mixer(p[6:10], pc[6:10], ret_g_out[l], not last)
        y_hg, yc_hg = hgrn2_mixer(p[10:15], pc[10:15], hg_lb[l], hg_g_out[l], not last)
        x = x + gt_a * (jnp.concatenate([y_mla, y_na, y_ret, y_hg], axis=-1) @ w_out[l])

        moe_prm = (moe_w_rg[l], moe_b_rg[l], moe_w_re[l], moe_b_re[l], moe_w1[l], moe_w3[l], moe_w2[l])
        if last:
            h = modulate(x, sh_m, sc_m).reshape(b * n, d)
            x = x + gt_m * hier_moe(h, *moe_prm).reshape(b, n, d)
        else:
            xc = xc + cgt_a * (jnp.concatenate([yc_mla, yc_na, yc_ret, yc_hg], axis=-1) @ w_out[l])
            h = jnp.concatenate([modulate(x, sh_m, sc_m).reshape(b * n, d),
                                 modulate(xc, csh_m, csc_m).reshape(b * n_ctx, d)], axis=0)
            out = hier_moe(h, *moe_prm)
            x = x + gt_m * out[:b * n].reshape(b, n, d)
            xc = xc + cgt_m * out[b * n:].reshape(b, n_ctx, d)
    return x
```

```python
import numpy as np
import concourse.bass as bass
import concourse.mybir as mybir
from concourse.alu_op_type import AluOpType as ALU
from concourse.bass_utils import run_bass_kernel_spmd

F32 = mybir.dt.float32
BF16 = mybir.dt.bfloat16
I32 = mybir.dt.int32
U32 = mybir.dt.uint32
AF = mybir.ActivationFunctionType
AX = mybir.AxisListType

D = 1024
NLAT = 4096
NCTX = 256
T = NLAT + NCTX
NT = T // 128
DEPTH = 4
PTOT = 3424
EPS = 1e-6
MLA_SCALE = 96.0 ** -0.5
NA_SCALE = 0.125
NEG = -30000.0
CAP = 1024
NEXP = 32
FF = 512
PROJ_INTERLEAVE = True


class View:
    __slots__ = ("buf", "ap")

    def __init__(self, buf, ap):
        self.buf = buf
        self.ap = ap


class Buf:
    def __init__(self, name, t, kind, shape, dt):
        self.name = name
        self.t = t
        self.kind = kind
        self.shape = list(shape)
        self.dt = dt
        self.w = {}
        self.r = {}
        self.dsem = None
        self.dcount = 0
        self.dkey = None
        self.F = int(np.prod(shape[1:]))

    def __getitem__(self, idx):
        return View(self, self.t[idx])

    def ap(self, off, dims, p0=0, npart=None):
        if npart is None:
            npart = self.shape[0] - p0
        if self.kind == "dram":
            return View(self, bass.AP(self.t, off, dims))
        return View(self, bass.AP(self.t, p0 * self.F + off, [[self.F, npart]] + [list(d) for d in dims]))


class Prog:
    def __init__(self, nc):
        self.nc = nc
        self.engs = {"pe": nc.tensor, "dve": nc.vector, "act": nc.scalar, "pool": nc.gpsimd, "sp": nc.sync}
        self.sem = {}
        self.cnt = {}
        self.waited = {k: {} for k in self.engs}
        self._stack = []
        self._semcms = []
        self.free_dsems = []
        self.nsem = 0
        self.dsems = []
        self.nwaits = 0
        self.nins = 0
        self.uid = 0
        self.rec = None
        for k in self.engs:
            self.sem[k] = self._newsem("e_" + k)
            self.cnt[k] = 0

    def _newsem(self, name):
        cm = self.nc.semaphore(name)
        s = cm.__enter__()
        self._semcms.append(cm)
        self.nsem += 1
        return s

    def mark(self):
        return len(self._stack)

    def release(self, mark):
        self.barrier()
        while len(self._stack) > mark:
            cm, buf = self._stack.pop()
            cm.__exit__(None, None, None)
            if buf is not None and buf.dsem is not None:
                self.free_dsems.append((buf.dsem, buf.dcount, buf.dkey))
                self.dsems.remove(buf)
                buf.dsem = None

    def sbuf(self, name, shape, dt):
        self.uid += 1
        name = "%s_%d" % (name, self.uid)
        cm = self.nc.sbuf_tensor(name, list(shape), dt)
        t = cm.__enter__()
        b = Buf(name, t, "sbuf", shape, dt)
        self._stack.append((cm, b))
        return b

    def psum(self, name, shape, dt=F32):
        self.uid += 1
        name = "%s_%d" % (name, self.uid)
        cm = self.nc.psum_tensor(name, list(shape), dt)
        t = cm.__enter__()
        b = Buf(name, t, "psum", shape, dt)
        self._stack.append((cm, b))
        return b

    def dram(self, name, shape, dt, kind="Internal"):
        t = self.nc.dram_tensor(name, list(shape), dt, kind=kind)
        return Buf(name, t, "dram", shape, dt)

    def _wait(self, eng, key, tok):
        sem, val = tok
        w = self.waited[eng]
        if w.get(key, 0) >= val:
            return
        self.engs[eng].wait_ge(sem, val)
        w[key] = val
        self.nwaits += 1

    def _deps(self, eng, reads, writes, pe_accum=False):
        for b in reads:
            for k, t in b.w.items():
                self._wait(eng, k, t)
        for b in writes:
            for k, t in b.w.items():
                if pe_accum and k == "e_pe":
                    continue
                self._wait(eng, k, t)
            for k, t in b.r.items():
                self._wait(eng, k, t)

    def record(self, fn):
        assert self.rec is None
        self.rec = []
        fn()
        r = self.rec
        self.rec = None
        return r

    def play(self, *streams):
        streams = [st for st in streams if st]
        pos = [0] * len(streams)
        total = max(len(st) for st in streams) if streams else 0
        for step in range(1, total + 1):
            for k, st in enumerate(streams):
                tgt = (len(st) * step + total - 1) // total
                while pos[k] < tgt:
                    st[pos[k]]()
                    pos[k] += 1

    def op(self, eng, fn, reads=(), writes=(), pe_accum=False):
        if self.rec is not None:
            self.rec.append(lambda: self._op(eng, fn, reads, writes, pe_accum))
            return None
        return self._op(eng, fn, reads, writes, pe_accum)

    def _op(self, eng, fn, reads=(), writes=(), pe_accum=False):
        reads = [v.buf for v in reads if isinstance(v, View)]
        writes = [v.buf for v in writes if isinstance(v, View)]
        self._deps(eng, reads, writes, pe_accum)
        ins = fn(self.engs[eng])
        self.cnt[eng] += 1
        self.nins += 1
        ins.then_inc(self.sem[eng], 1)
        key = "e_" + eng
        tok = (self.sem[eng], self.cnt[eng])
        for b in reads:
            b.r[key] = tok
        for b in writes:
            b.w[key] = tok
        return ins

    def dma(self, q, out, in_, extra_reads=(), indirect=None, **kw):
        if self.rec is not None:
            self.rec.append(lambda: self._dma(q, out, in_, extra_reads, indirect, **kw))
            return None
        return self._dma(q, out, in_, extra_reads, indirect, **kw)

    def _dma(self, q, out, in_, extra_reads=(), indirect=None, **kw):
        dst, src = out.buf, in_.buf
        sb = dst if dst.kind == "sbuf" else src
        assert sb.kind == "sbuf"
        if sb.dsem is None:
            if self.free_dsems:
                sb.dsem, sb.dcount, sb.dkey = self.free_dsems.pop()
            else:
                sb.dkey = "ds%d" % self.nsem
                sb.dsem = self._newsem(sb.dkey)
                sb.dcount = 0
            self.dsems.append(sb)
        rd = [src] + [v.buf for v in extra_reads]
        self._deps(q, rd, [dst])
        if indirect is None:
            ins = self.engs[q].dma_start(out=out.ap, in_=in_.ap, **kw)
        else:
            ins = indirect(self.engs[q])
        sb.dcount += 16
        self.nins += 1
        ins.then_inc(sb.dsem, 16)
        key = sb.dkey
        tok = (sb.dsem, sb.dcount)
        for b in rd:
            b.r[key] = tok
        dst.w[key] = tok
        return ins

    def barrier(self):
        for eng in self.engs:
            for k in self.engs:
                if self.cnt[k] and k != eng:
                    self._wait(eng, "e_" + k, (self.sem[k], self.cnt[k]))
            for sb in self.dsems:
                if sb.dcount:
                    self._wait(eng, sb.dkey, (sb.dsem, sb.dcount))

    def close(self):
        while self._stack:
            self._stack.pop()[0].__exit__(None, None, None)
        while self._semcms:
            self._semcms.pop().__exit__(None, None, None)

    def mm(self, out, lhsT, rhs, start=True, stop=True):
        return self.op("pe", lambda e: e.matmul(out.ap, lhsT.ap, rhs.ap, start=start, stop=stop),
                       [lhsT, rhs], [out], pe_accum=True)

    def tr(self, out, in_, ident):
        return self.op("pe", lambda e: e.transpose(out.ap, in_.ap, ident.ap), [in_, ident], [out], pe_accum=True)

    def act(self, out, in_, func, bias=None, scale=None, accum=None, eng="act"):
        kw = {}
        rd = [in_]
        wr = [out]
        if bias is not None:
            kw["bias"] = bias.ap if isinstance(bias, View) else bias
            rd.append(bias)
        if scale is not None:
            kw["scale"] = scale.ap if isinstance(scale, View) else scale
            rd.append(scale)
        if accum is not None:
            kw["accum_out"] = accum.ap
            wr.append(accum)
        return self.op("act", lambda e: e.activation(out.ap, in_.ap, func, **kw), rd, wr)

    def tt(self, out, a, b, op, eng="dve"):
        return self.op(eng, lambda e: e.tensor_tensor(out.ap, a.ap, b.ap, op), [a, b], [out])

    def ts(self, out, a, s1, s2, op0, op1=None, eng="dve"):
        rd = [a, s1, s2]
        x1 = s1.ap if isinstance(s1, View) else s1
        x2 = s2.ap if isinstance(s2, View) else s2
        if op1 is None:
            return self.op(eng, lambda e: e.tensor_scalar(out.ap, a.ap, x1, None, op0), rd, [out])
        return self.op(eng, lambda e: e.tensor_scalar(out.ap, a.ap, x1, x2, op0, op1), rd, [out])

    def stt(self, out, a, s, b, op0, op1):
        x = s.ap if isinstance(s, View) else s
        return self.op("dve", lambda e: e.scalar_tensor_tensor(out.ap, a.ap, x, b.ap, op0, op1), [a, s, b], [out])

    def red(self, out, a, op=ALU.add, axis=AX.X):
        return self.op("dve", lambda e: e.tensor_reduce(out.ap, a.ap, axis, op), [a], [out])

    def recip(self, out, a):
        return self.op("dve", lambda e: e.reciprocal(out.ap, a.ap), [a], [out])

    def copy(self, out, a, eng="dve"):
        if eng == "act":
            return self.op("act", lambda e: e.copy(out.ap, a.ap), [a], [out])
        return self.op(eng, lambda e: e.tensor_copy(out.ap, a.ap), [a], [out])

    def memset(self, out, val, eng="dve"):
        return self.op(eng, lambda e: e.memset(out.ap, val), [], [out])


def _rope_tables():
    pos = np.arange(NLAT)
    rows = (pos // 64).astype(np.float32)
    cols = (pos % 64).astype(np.float32)
    inv = (10000.0 ** (-np.arange(0, 16, 2, dtype=np.float32) / 16)).astype(np.float32)
    ang = np.concatenate([rows[:, None] * inv, cols[:, None] * inv], axis=-1).astype(np.float32)
    c, s = np.cos(ang).astype(np.float32), np.sin(ang).astype(np.float32)
    c1, c2, s1, s2 = c[:, :8], c[:, 8:], s[:, :8], s[:, 8:]
    C32 = np.concatenate([c1, c1, c2, c2], axis=1)
    S32 = np.concatenate([-s1, s1, -s2, s2], axis=1)
    C32 = np.concatenate([C32, np.ones((NCTX, 32), np.float32)], axis=0)
    S32 = np.concatenate([S32, np.zeros((NCTX, 32), np.float32)], axis=0)
    return np.ascontiguousarray(np.concatenate([C32, S32], axis=1).astype(np.float32))


def _consts():
    c = {}
    c["ident"] = np.eye(128, dtype=np.float32)
    c["rope"] = _rope_tables()
    return c


class Ctx:
    pass


def declare_io(P, g):
    io = Ctx()
    ext = lambda n, s: P.dram(n, s, F32, kind="ExternalInput")
    io.x = ext("x", [NLAT, D])
    io.ctx = ext("ctx", [NCTX, D])
    io.cvT = ext("cvT", [128, 8, 2])
    io.w_ada = ext("w_ada", [DEPTH, D, 6 * D])
    io.b_ada = ext("b_ada", [DEPTH, 6 * D])
    io.w_in = ext("w_in", [DEPTH, D, PTOT])
    io.w_out = ext("w_out", [DEPTH, D, D])
    io.gains = ext("gains", [DEPTH, 768])
    io.w_uq = ext("mla_w_uq", [DEPTH, 192, 384])
    io.w_ukv = ext("mla_w_ukv", [DEPTH, 128, 512])
    io.ident = ext("ident", [128, 128])
    io.rope = ext("rope", [T, 64])
    io.hg_lb_raw = ext("hg_lb_raw", [DEPTH, 256])
    io.na_bias = ext("na_bias", [DEPTH, 128, 100, 128])
    io.gla_mask = ext("gla_mask", [128, 512])
    io.gla_mm = ext("gla_mm", [128, 768])
    io.gla_ci = ext("gla_ci", [128, 16])
    io.ret_tab = ext("ret_tab", [128, 2048])
    io.ret_dec = ext("ret_dec", [64, 16])
    io.moe_wr = ext("moe_wr", [DEPTH, D, 36])
    io.moe_br = ext("moe_br", [DEPTH, 36])
    io.moe_U = ext("moe_U", [128, 128])
    io.moe_eoff = ext("moe_eoff", [128, NEXP])
    io.moe_w1 = ext("moe_w1", [DEPTH, NEXP, D, FF])
    io.moe_w3 = ext("moe_w3", [DEPTH, NEXP, D, FF])
    io.moe_w2 = ext("moe_w2", [DEPTH, NEXP, FF, D])
    return io


def alloc_scratch(P):
    s = Ctx()
    s.xres = P.dram("xres", [T, D], F32)
    s.mlaQT = P.dram("mlaQT", [4, 96, T], BF16)
    s.mlaKT = P.dram("mlaKT", [4, 96, T], BF16)
    s.naQT = P.dram("naQT", [256, T], BF16)
    s.naKT = P.dram("naKT", [256, T], BF16)
    s.VV = P.dram("VV", [T, 1024], BF16)
    s.GL = P.dram("GL", [T, 2304], F32)
    s.YT = P.dram("YT", [512, T], BF16)
    s.OF = P.dram("OF", [T, 512], F32)
    s.MOD = P.dram("MOD", [2, 6 * D], F32)
    s.XS = P.dram("XS", [NEXP * CAP, D], BF16)
    s.YS = P.dram("YS", [NEXP * CAP, D], BF16)
    s.Y = P.dram("Y", [T, 512], BF16)
    return s


def rms_groups(P, W, src, G, n, gain, out, scale=1.0):
    pool = getattr(W, "pool", None)
    if pool:
        W.pi = (getattr(W, "pi", 0) + 1) % len(pool)
        Wsq, Wst = pool[W.pi]
    else:
        Wsq, Wst = W.sq, W.st
    sq = Wsq.ap(0, [[n, G], [1, n]])
    P.act(sq, src, AF.Square)
    ss = Wst.ap(0, [[1, G]])
    P.red(ss, sq)
    ms = Wst.ap(16, [[1, G]])
    P.ts(ms, ss, 1.0 / n, EPS, ALU.mult, ALU.add)
    P.act(ms, ms, AF.Sqrt)
    rs = Wst.ap(32, [[1, G]])
    P.recip(rs, ms)
    if scale != 1.0:
        P.ts(rs, rs, float(scale), None, ALU.mult)
    rsb = Wst.ap(32, [[1, G], [0, n]])
    P.tt(sq, src, rsb, ALU.mult)
    P.tt(out, sq, gain, ALU.mult)


class ModSlice:
    def __init__(self, P, sc, v, c0, c1):
        self.c0 = c0
        self.buf = P.sbuf("mod", [128, c1 - c0], F32)
        P.dma("sp", self.buf[:], sc.MOD.ap(v * 6 * D + c0, [[0, 128], [1, c1 - c0]]))

    def __getitem__(self, idx):
        rows, cols = idx
        return self.buf[rows, cols.start - self.c0:cols.stop - self.c0]


def phase_mod(P, io, sc, l):
    m = P.mark()
    cv = P.sbuf("cv", [128, 8, 2], F32)
    cvs = P.sbuf("cvs", [128, 8, 2], BF16)
    P.dma("sp", cv[:], io.cvT[:])
    P.act(cvs[:], cv[:], AF.Silu)
    wb = [P.sbuf("wada", [128, 8, 512], BF16) for _ in range(2)]
    bb = [P.sbuf("bada", [128, 512], F32) for _ in range(2)]
    mt = [P.sbuf("modt", [128, 512], F32) for _ in range(4)]
    ps = [P.psum("pmod", [128, 512]) for _ in range(2)]
    for g in range(12):
        w = wb[g % 2]
        b = bb[g % 2]
        P.dma("pool", w[:], io.w_ada.ap(l * D * 6 * D + g * 512, [[6 * D, 128], [128 * 6 * D, 8], [1, 512]]))
        P.dma("sp", b[:], io.b_ada.ap(l * 6 * D + g * 512, [[0, 128], [1, 512]]))
        for v in range(2):
            p = ps[v]
            for k in range(8):
                lhsT = cvs.ap(k * 2 + v, [[0, 128]])
                P.mm(p[:], lhsT, w[:, k, :], start=(k == 0), stop=(k == 7))
            dst = mt[(g * 2 + v) % 4]
            if g in (2, 3, 8, 9):
                P.stt(dst[:], p[:], 1.0, b[:], ALU.add, ALU.add)
            else:
                P.tt(dst[:], p[:], b[:], ALU.add)
            P.dma("sp", sc.MOD[v:v + 1, g * 512:(g + 1) * 512], dst[0:1, :])
    P.release(m)


def phase_proj(P, io, sc, l, lbt, dbg=None):
    m = P.mark()
    modL = ModSlice(P, sc, 0, 0, 2048)
    modC = ModSlice(P, sc, 1, 0, 2048)
    W = Ctx()
    W.st = P.sbuf("st", [128, 64], F32)
    ident_f = P.sbuf("identf", [128, 128], F32)
    ident = P.sbuf("ident", [128, 128], BF16)
    P.dma("sp", ident_f[:], io.ident[:])
    P.copy(ident[:], ident_f[:])
    win = P.sbuf("win", [128, 8, PTOT], BF16)
    for k in range(8):
        P.dma("pool", win[:, k, :], io.w_in.ap(l * D * PTOT + k * 128 * PTOT, [[PTOT, 128], [1, PTOT]]))
    wuq_a = P.sbuf("wuqa", [128, 384], BF16)
    wuq_b = P.sbuf("wuqb", [64, 384], BF16)
    wukv = P.sbuf("wukv", [128, 512], BF16)
    P.dma("pool", wuq_a[:], io.w_uq.ap(l * 192 * 384, [[384, 128], [1, 384]]))
    P.dma("pool", wuq_b[:], io.w_uq.ap(l * 192 * 384 + 128 * 384, [[384, 64], [1, 384]]))
    P.dma("pool", wukv[:], io.w_ukv.ap(l * 128 * 512, [[512, 128], [1, 512]]))
    gn = P.sbuf("gains", [128, 640], F32)
    P.dma("sp", gn[:], io.gains.ap(l * 768, [[0, 128], [1, 640]]))

    W.pool = [(P.sbuf("sqp", [128, 256], F32), P.sbuf("stp", [128, 64], F32)) for _ in range(4)]
    Wb = Ctx()
    Wb.pool = [(P.sbuf("sqb", [128, 256], F32), P.sbuf("stb", [128, 64], F32)) for _ in range(2)]
    psNT = P.psum("psNT", [128, 4, 128], BF16)
    vva = [P.sbuf("vva", [128, 256], BF16) for _ in range(2)]
    xt = [P.sbuf("xt", [128, D], F32) for _ in range(2)]
    xsq = P.sbuf("xsq", [128, D], F32)
    sst = P.sbuf("sst", [128, 2], F32)
    xn = P.sbuf("xn", [128, D], BF16)
    xnT = P.sbuf("xnT", [128, 8, 128], BF16)
    psT = P.psum("psT", [128, 8, 128], BF16)
    psP = [P.psum("psP", [128, 512]) for _ in range(2)]
    pps = [P.sbuf("pp", [128, PTOT], F32) for _ in range(2)]
    ropes = [P.sbuf("rope", [128, 64], F32) for _ in range(2)]
    lat = P.sbuf("lat", [128, 352], BF16)
    latT = P.sbuf("latT", [128, 3, 128], BF16)
    psL = P.psum("psL", [128, 3, 128], BF16)
    psQ = P.psum("psQ", [128, 512])
    psKV = P.psum("psKV", [128, 512])
    qf = P.sbuf("qf", [128, 4, 96], F32)
    kf = P.sbuf("kf", [128, 4, 96], F32)
    krn = P.sbuf("krn", [128, 32], F32)
    tmpr = P.sbuf("tmpr", [128, 4, 32], F32)
    tmpk = P.sbuf("tmpk", [128, 32], F32)
    qb = P.sbuf("qb", [128, 4, 96], BF16)
    kb = P.sbuf("kb", [128, 4, 96], BF16)
    psQT = P.psum("psQT", [128, 8, 128], BF16)
    qkT = [P.sbuf("qkT", [96, 8, 128], BF16) for _ in range(2)]
    naqk = P.sbuf("naqk", [128, 512], BF16)
    naT = [P.sbuf("naT", [128, 4, 128], BF16) for _ in range(2)]
    vvs = [P.sbuf("vv", [128, 1024], BF16) for _ in range(2)]
    gls = [P.sbuf("gl", [128, 2304], F32) for _ in range(2)]
    tmpf = P.sbuf("tmpf", [128, 512], F32)
    W.fb = P.sbuf("fbuf", [128, 512], F32)

    groups = [(0, 352), (352, 512), (864, 512), (1376, 512), (1888, 512), (2400, 512), (2912, 512)]

    def stage1(i):
        isctx = i >= 32
        mod = modC if isctx else modL
        x = xt[i % 2]
        pp = pps[i % 2]
        if l == 0:
            src = io.ctx[(i - 32) * 128:(i - 31) * 128, :] if isctx else io.x[i * 128:(i + 1) * 128, :]
        else:
            src = sc.xres[i * 128:(i + 1) * 128, :]
        P.dma("sp", x[:], src)
        P.dma("sp", ropes[i % 2][:], io.rope[i * 128:(i + 1) * 128, :])
        ssx = sst[:, 0:1]
        P.act(xsq[:], x[:], AF.Square, accum=ssx)
        P.ts(ssx, ssx, 1.0 / D, EPS, ALU.mult, ALU.add)
        P.act(ssx, ssx, AF.Sqrt)
        P.recip(ssx, ssx)
        P.stt(xsq[:], x[:], ssx, mod[:, 1024:2048], ALU.mult, ALU.mult)
        P.tt(xn[:], xsq[:], mod[:, 0:1024], ALU.add)
        if l == 0:
            P.dma("sp", sc.xres[i * 128:(i + 1) * 128, :], x[:])
        for k in range(8):
            P.tr(psT[:, k, :], xn[:, k * 128:(k + 1) * 128], ident[:])
        P.copy(xnT[:, 0:4, :], psT[:, 0:4, :], eng="act")
        P.copy(xnT[:, 4:8, :], psT[:, 4:8, :], eng="dve")
        for gi, (c0, n) in enumerate(groups):
            ps = psP[gi % 2]
            for k in range(8):
                P.mm(ps[:, 0:n], xnT[:, k, :], win[:, k, c0:c0 + n], start=(k == 0), stop=(k == 7))
            P.copy(pp[:, c0:c0 + n], ps[:, 0:n], eng=("act" if gi % 2 == 0 else "dve"))
        if dbg is not None and "pp" in dbg:
            P.dma("sp", dbg["pp"][i * 128:(i + 1) * 128, :], pp[:])

    def stage2a(i):
        pp = pps[i % 2]
        rope = ropes[i % 2]
        vv = vvs[i % 2]
        gl = gls[i % 2]
        rms_groups(P, W, pp.ap(0, [[192, 1], [1, 192]]), 1, 192, gn.ap(0, [[0, 1], [1, 192]]),
                   lat.ap(0, [[192, 1], [1, 192]]))
        rms_groups(P, W, pp.ap(192, [[128, 1], [1, 128]]), 1, 128, gn.ap(192, [[0, 1], [1, 128]]),
                   lat.ap(192, [[128, 1], [1, 128]]))
        P.tr(psL[:, 0, :], lat[:, 0:128], ident[:])
        P.tr(psL[0:64, 1, :], lat[:, 128:192], ident[:])
        P.tr(psL[:, 2, :], lat[:, 192:320], ident[:])
        P.copy(latT[:, 0, :], psL[:, 0, :], eng="act")
        P.copy(latT[0:64, 1, :], psL[0:64, 1, :], eng="dve")
        P.copy(latT[:, 2, :], psL[:, 2, :], eng="act")
        P.mm(psQ[:, 0:384], latT[:, 0, :], wuq_a[:], start=True, stop=False)
        P.mm(psQ[:, 0:384], latT[0:64, 1, :], wuq_b[:], start=False, stop=True)
        P.mm(psKV[:], latT[:, 2, :], wukv[:], start=True, stop=True)
        rms_groups(P, W, psQ.ap(0, [[96, 4], [1, 64]]), 4, 64, gn.ap(320, [[0, 4], [1, 64]]),
                   qf.ap(0, [[96, 4], [1, 64]]), scale=MLA_SCALE)
        rms_groups(P, W, psQ.ap(64, [[96, 4], [1, 32]]), 4, 32, gn.ap(384, [[0, 4], [1, 32]]),
                   qf.ap(64, [[96, 4], [1, 32]]), scale=MLA_SCALE)
        rms_groups(P, W, psKV.ap(0, [[128, 4], [1, 64]]), 4, 64, gn.ap(416, [[0, 4], [1, 64]]),
                   kf.ap(0, [[96, 4], [1, 64]]))
        rms_groups(P, W, pp.ap(320, [[32, 1], [1, 32]]), 1, 32, gn.ap(480, [[0, 1], [1, 32]]),
                   krn.ap(0, [[32, 1], [1, 32]]))
        P.copy(vva[i % 2].ap(0, [[64, 4], [1, 64]]), psKV.ap(64, [[128, 4], [1, 64]]), eng="act")
        P.dma("sp", sc.VV[i * 128:(i + 1) * 128, 0:256], vva[i % 2][:])
        qr_ = lambda o: qf.ap(64 + o, [[96, 4], [16, 2], [1, 8]])
        tm_ = lambda o: tmpr.ap(o, [[32, 4], [16, 2], [1, 8]])
        P.copy(tm_(0), qr_(8))
        P.copy(tm_(8), qr_(0))
        Cb = rope.ap(0, [[0, 4], [1, 32]])
        Sb = rope.ap(32, [[0, 4], [1, 32]])
        P.tt(tmpr[:], tmpr[:], Sb, ALU.mult, eng="pool")
        P.tt(qf.ap(64, [[96, 4], [1, 32]]), qf.ap(64, [[96, 4], [1, 32]]), Cb, ALU.mult)
        P.tt(qf.ap(64, [[96, 4], [1, 32]]), qf.ap(64, [[96, 4], [1, 32]]), tmpr[:], ALU.add)
        P.copy(tmpk.ap(0, [[16, 2], [1, 8]]), krn.ap(8, [[16, 2], [1, 8]]))
        P.copy(tmpk.ap(8, [[16, 2], [1, 8]]), krn.ap(0, [[16, 2], [1, 8]]))
        P.tt(tmpk[:], tmpk[:], rope[:, 32:64], ALU.mult, eng="pool")
        P.tt(krn[:], krn[:], rope[:, 0:32], ALU.mult)
        P.tt(krn[:], krn[:], tmpk[:], ALU.add)
        P.copy(kf.ap(64, [[96, 4], [1, 32]]), krn.ap(0, [[0, 4], [1, 32]]))
        P.copy(qb[:], qf[:], eng="act")
        P.copy(kb[:], kf[:], eng="act")
        for h in range(4):
            P.tr(psQT[0:96, h, :], qb[:, h, :], ident[:])
            P.tr(psQT[0:96, 4 + h, :], kb[:, h, :], ident[:])
        qkTt = qkT[i % 2]
        P.copy(qkTt[:, 0:4, :], psQT[0:96, 0:4, :], eng="act")
        P.copy(qkTt[:, 4:8, :], psQT[0:96, 4:8, :], eng="dve")
        P.dma("sp", sc.mlaQT.ap(i * 128, [[T, 96], [96 * T, 4], [1, 128]]), qkTt[:, 0:4, :])
        P.dma("sp", sc.mlaKT.ap(i * 128, [[T, 96], [96 * T, 4], [1, 128]]), qkTt[:, 4:8, :])

    def stage2b(i):
        pp = pps[i % 2]
        rope = ropes[i % 2]
        vv = vvs[i % 2]
        gl = gls[i % 2]
        rms_groups(P, Wb, pp.ap(352, [[64, 4], [1, 64]]), 4, 64, gn.ap(512, [[0, 4], [1, 64]]),
                   naqk.ap(0, [[64, 4], [1, 64]]), scale=NA_SCALE)
        rms_groups(P, Wb, pp.ap(608, [[64, 4], [1, 64]]), 4, 64, gn.ap(576, [[0, 4], [1, 64]]),
                   naqk.ap(256, [[64, 4], [1, 64]]))
        for j in range(4):
            P.tr(psNT[:, j, :], naqk[:, j * 128:(j + 1) * 128], ident[:])
        naTt = naT[i % 2]
        P.copy(naTt[:, 0:2, :], psNT[:, 0:2, :], eng="act")
        P.copy(naTt[:, 2:4, :], psNT[:, 2:4, :], eng="dve")
        P.dma("sp", sc.naQT.ap(i * 128, [[T, 128], [128 * T, 2], [1, 128]]), naTt[:, 0:2, :])
        P.dma("sp", sc.naKT.ap(i * 128, [[T, 128], [128 * T, 2], [1, 128]]), naTt[:, 2:4, :])
        P.copy(vv[:, 256:512], pp[:, 864:1120], eng="act")
        P.copy(vv[:, 512:768], pp[:, 1632:1888], eng="dve")
        P.copy(vv[:, 768:1024], pp[:, 2912:3168], eng="act")
        P.dma("sp", sc.VV[i * 128:(i + 1) * 128, 256:1024], vv[:, 256:1024])

    def stage2c(i):
        pp = pps[i % 2]
        rope = ropes[i % 2]
        vv = vvs[i % 2]
        gl = gls[i % 2]
        P.copy(gl[:, 0:256], pp[:, 1120:1376], eng="dve")
        P.ts(gl[:, 256:512], pp[:, 1376:1632], 0.125, None, ALU.mult)
        P.act(gl[:, 512:768], pp[:, 2144:2400], AF.Silu)
        P.act(tmpf[:], pp[:, 2400:2912], AF.Sigmoid)
        hg_gates(P, W, lbt, l, tmpf, gl)
        P.act(gl[:, 1792:2048], pp[:, 1888:2144], AF.Silu)
        P.act(gl[:, 2048:2304], pp[:, 3168:3424], AF.Silu)
        P.dma("sp", sc.GL[i * 128:(i + 1) * 128, :], gl[:])


    stage1(0)
    for i in range(NT):
        sa = P.record(lambda: stage2a(i))
        sb_ = P.record(lambda: stage2b(i))
        sc_ = P.record(lambda: stage2c(i))
        s1 = P.record(lambda: stage1(i + 1)) if i + 1 < NT else []
        P.play(sa, sb_, sc_, s1)
    P.release(m)


def hg_gates(P, W, lbt, l, sig, gl):
    for d in range(2):
        s = sig[:, d * 256:(d + 1) * 256]
        f = W.fb[:, d * 256:(d + 1) * 256]
        P.tt(f, s, lbt[:, l, 256:512], ALU.mult)
        P.tt(f, f, lbt[:, l, 0:256], ALU.add)
        P.ts(gl[:, 768 + d * 256:1024 + d * 256], f, -1.0, 1.0, ALU.mult, ALU.add)
        P.ts(f, f, 1e-20, None, ALU.max)
        P.act(gl[:, 1280 + d * 256:1536 + d * 256], f, AF.Ln)


def phase_lb(P, io, lbt):
    m = P.mark()
    raw = P.sbuf("lbraw", [128, 4, 256], F32)
    e = P.sbuf("lbe", [128, 4, 256], F32)
    mx = P.sbuf("lbmx", [128, 256], F32)
    P.dma("sp", raw[:], io.hg_lb_raw.ap(0, [[0, 128], [256, 4], [1, 256]]))
    P.tt(mx[:], raw[:, 0, :], raw[:, 1, :], ALU.max)
    P.tt(mx[:], mx[:], raw[:, 2, :], ALU.max)
    P.tt(mx[:], mx[:], raw[:, 3, :], ALU.max)
    for l in range(4):
        P.tt(e[:, l, :], raw[:, l, :], mx[:], ALU.subtract)
    P.act(e[:], e[:], AF.Exp)
    P.tt(mx[:], e[:, 0, :], e[:, 1, :], ALU.add)
    P.tt(mx[:], mx[:], e[:, 2, :], ALU.add)
    P.tt(mx[:], mx[:], e[:, 3, :], ALU.add)
    P.recip(mx[:], mx[:])
    for l in range(4):
        P.tt(e[:, l, :], e[:, l, :], mx[:], ALU.mult)
    P.memset(lbt[:, 0, 0:256], 0.0)
    P.copy(lbt[:, 1, 0:256], e[:, 1, :])
    P.tt(lbt[:, 2, 0:256], lbt[:, 1, 0:256], e[:, 2, :], ALU.add)
    P.tt(lbt[:, 3, 0:256], lbt[:, 2, 0:256], e[:, 3, :], ALU.add)
    for l in range(4):
        P.ts(lbt[:, l, 256:512], lbt[:, l, 0:256], -1.0, 1.0, ALU.mult, ALU.add)
    P.release(m)


def attn_epilogue(P, A, OT, n, yrow0, q0, sc):
    osb = A.osb[A.ei % 2]
    yb = A.yb[A.ei % 2]
    A.ei += 1
    P.copy(osb[:, 0:n], OT[:, 0:n], eng="dve")
    P.mm(A.psB[:, 0:n], A.ones[64:65, 0:64], osb[64:65, 0:n], start=True, stop=True)
    P.recip(A.rb[:, 0:n], A.psB[:, 0:n])
    P.tt(yb[:, 0:n], osb[0:64, 0:n], A.rb[:, 0:n], ALU.mult)
    P.dma("sp", sc.YT[yrow0:yrow0 + 64, q0:q0 + n], yb[:, 0:n])


def attn_common(P):
    A = Ctx()
    A.ei = 0
    A.osb = [P.sbuf("osb", [65, 512], F32) for _ in range(2)]
    A.yb = [P.sbuf("yb", [64, 512], BF16) for _ in range(2)]
    A.rb = P.sbuf("rb", [64, 512], F32)
    A.ones = P.sbuf("ones", [65, 64], F32)
    P.memset(A.ones[:], 1.0)
    A.psB = P.psum("psB", [64, 512])
    A.vt = P.sbuf("vt", [128, NT, 4, 65], BF16)
    return A


def load_v(P, A, sc, col0):
    P.memset(A.vt.ap(64, [[65, NT * 4], [1, 1]]), 1.0)
    for i in range(NT):
        P.dma("sp", A.vt[:, i, :, 0:64], sc.VV.ap(i * 128 * 1024 + col0, [[1024, 128], [64, 4], [1, 64]]))


def phase_mla(P, io, sc):
    m = P.mark()
    A = attn_common(P)
    load_v(P, A, sc, 0)
    KT = P.sbuf("KT", [96, 4, T], BF16)
    QT = P.sbuf("QT", [96, 4, T], BF16)
    for h in range(4):
        P.dma("sp", KT[:, h, :], sc.mlaKT[h, :, :])
        P.dma("sp", QT[:, h, :], sc.mlaQT[h, :, :])
    NB = 5
    LAG = 3
    psS = [P.psum("psS", [128, 512]) for _ in range(NB)]
    psO = [P.psum("psO", [65, 512]) for _ in range(2)]
    PT = [P.sbuf("PT", [128, 512], BF16) for _ in range(NB)]
    it = 0
    ci = 0
    for h in range(4):
        for qc in range(9):
            q0 = qc * 512
            n = 512 if qc < 8 else 256
            kts = list(range(NT)) if qc < 8 else [32, 33]
            OT = psO[ci % 2]
            ci += 1
            nk = len(kts)
            slots = []
            for step in range(nk + LAG):
                if step < nk:
                    kt = kts[step]
                    S = psS[it % NB]
                    pt = PT[it % NB]
                    it += 1
                    slots.append(pt)
                    P.mm(S[:, 0:n], KT[:, h, kt * 128:(kt + 1) * 128], QT[:, h, q0:q0 + n])
                    P.act(pt[:, 0:n], S[:, 0:n], AF.Exp)
                j = step - LAG
                if j >= 0:
                    P.mm(OT[:, 0:n], A.vt[:, kts[j], h, :], slots[j][:, 0:n], start=(j == 0), stop=(j == nk - 1))
            attn_epilogue(P, A, OT, n, h * 64, q0, sc)
    P.release(m)


def na_bias_index():
    dr = np.zeros((5, 5, 128, 128), np.int64)
    dc = np.zeros((5, 5, 128, 128), np.int64)
    va = np.zeros((5, 5, 128, 128), bool)
    qq = np.arange(128)
    kk = np.arange(128)
    for pat, i in enumerate([2, 0, 1, 30, 31]):
        kb = min(max(i - 2, 0), 27)
        for j in range(5):
            r = 2 * i + qq[:, None] // 64
            wq = qq[:, None] % 64
            krow = 2 * (kb + j) + kk[None, :] // 64
            wk = kk[None, :] % 64
            rs = np.clip(r - 4, 0, 56)
            cs = np.clip(wq - 8, 0, 48)
            v = (krow >= rs) & (krow < rs + 8) & (wk >= cs) & (wk < cs + 16)
            va[pat, j] = v
            dr[pat, j] = np.clip(krow - r + 7, 0, 14)
            dc[pat, j] = np.clip(wk - wq, -15, 15) + 15
    return dr, dc, va


def na_bias_tables(rpb):
    dr, dc, va = na_bias_index()
    L = rpb.shape[0]
    out = np.empty((L, 5, 4, 5, 128, 128), np.float32)
    for l in range(L):
        for h in range(4):
            out[l, :, h] = np.where(va, rpb[l, h][dr, dc], np.float32(NEG))
    return np.ascontiguousarray(out.transpose(0, 4, 1, 2, 3, 5).reshape(L, 128, 100, 128))


def phase_na(P, io, sc, l):
    m = P.mark()
    A = attn_common(P)
    load_v(P, A, sc, 256)
    KT = P.sbuf("nKT", [128, 2, T], BF16)
    QT = P.sbuf("nQT", [128, 2, T], BF16)
    for hp in range(2):
        P.dma("sp", KT[:, hp, :], sc.naKT[hp * 128:(hp + 1) * 128, :])
        P.dma("sp", QT[:, hp, :], sc.naQT[hp * 128:(hp + 1) * 128, :])
    BT = P.sbuf("BT", [128, 100, 128], BF16)
    for c in range(4):
        P.dma("pool", BT[:, c * 25:(c + 1) * 25, :], io.na_bias.ap(l * 128 * 12800 + c * 25 * 128, [[12800, 128], [128, 25], [1, 128]]))
    identf = P.sbuf("identf", [128, 128], F32)
    ident = P.sbuf("ident", [128, 128], BF16)
    P.dma("sp", identf[:], io.ident[:])
    P.copy(ident[:], identf[:])
    psS = [P.psum("psS", [128, 8, 128]) for _ in range(2)]
    psO = [P.psum("psO", [65, 512]) for _ in range(2)]
    PT = [P.sbuf("PT", [128, 8, 128], BF16) for _ in range(2)]
    it = 0
    pend = []

    def flush():
        while pend:
            pend.pop(0)()

    for h in range(4):
        pb = (h % 2) * 64
        hp = h // 2
        for grp in range(9):
            OT = psO[(h * 9 + grp) % 2]
            nsub = 4 if grp < 8 else 2
            for s in range(nsub):
                i = grp * 4 + s
                q0 = i * 128
                S = psS[it % 2]
                pt = PT[it % 2]
                it += 1
                if grp < 8:
                    kb = min(max(i - 2, 0), 27)
                    pat = {0: 1, 1: 2, 30: 3, 31: 4}.get(i, 0)
                    kts = [kb + j for j in range(5)] + [32, 33]
                    for j in range(5):
                        kt = kb + j
                        P.mm(S[:, j, :], KT[pb:pb + 64, hp, kt * 128:(kt + 1) * 128], QT[pb:pb + 64, hp, q0:q0 + 128],
                             start=True, stop=False)
                        P.mm(S[:, j, :], BT[:, (pat * 4 + h) * 5 + j, :], ident[:], start=False, stop=True)
                    for j in (5, 6):
                        kt = 32 + j - 5
                        P.mm(S[:, j, :], KT[pb:pb + 64, hp, kt * 128:(kt + 1) * 128], QT[pb:pb + 64, hp, q0:q0 + 128])
                else:
                    kts = [32, 33]
                    for j in range(2):
                        kt = 32 + j
                        P.mm(S[:, j, :], KT[pb:pb + 64, hp, kt * 128:(kt + 1) * 128], QT[pb:pb + 64, hp, q0:q0 + 128])
                nk = len(kts)
                P.act(pt[:, 0:nk, :], S[:, 0:nk, :], AF.Exp)
                flush()

                def pv(OT=OT, s=s, kts=kts, pt=pt, h=h, nk=nk, last=(s == nsub - 1), nsub=nsub, grp=grp):
                    for j, kt in enumerate(kts):
                        P.mm(OT[:, s * 128:(s + 1) * 128], A.vt[:, kt, h, :], pt[:, j, :], start=(j == 0), stop=(j == nk - 1))
                    if last:
                        attn_epilogue(P, A, OT, nsub * 128, 256 + h * 64, grp * 512, sc)
                pend.append(pv)
    flush()
    P.release(m)


NCH = (2, 4)


def gla_consts():
    s = np.arange(128)[:, None]
    t = np.arange(128)[None, :]
    c = {}
    masks = np.zeros((2, 2, 128, 128), np.float32)
    ci = np.zeros((128, 2, 8), np.float32)
    for mx in range(2):
        cs_ = 128 // NCH[mx]
        same = (s // cs_) == (t // cs_)
        masks[mx, 0] = same & (s <= t)
        masks[mx, 1] = same & (s >= t)
        for cc in range(NCH[mx]):
            ci[cc * cs_:(cc + 1) * cs_, mx, cc] = 1
    c["gla_mask"] = np.ascontiguousarray(masks.transpose(2, 0, 1, 3).reshape(128, 4 * 128))
    c["gla_ci"] = np.ascontiguousarray(ci.reshape(128, 16))
    cs_ = 128 // NCH[1]
    same = (s // cs_) == (t // cs_)
    mm = np.zeros((2, 3, 128, 128), np.float32)
    for d in range(2):
        le = (s <= t) if d == 0 else (s >= t)
        mid = (t // cs_) * cs_ + (cs_ // 2 - 1 if d == 0 else cs_ // 2)
        lemid = (s <= mid) if d == 0 else (s >= mid)
        MA = (same & le).astype(np.float32)
        Mm = (same & lemid).astype(np.float32)
        M3 = (same & ~le).astype(np.float32)
        mm[d, 0] = MA - Mm
        mm[d, 1] = MA
        mm[d, 2] = M3
    c["gla_mm"] = np.ascontiguousarray(mm.transpose(2, 0, 1, 3).reshape(128, 6 * 128))
    j = np.arange(8, dtype=np.float64)
    lg = np.log1p(-np.exp2(-5.0 - j))
    lgd = [lg[0::2], lg[1::2]]
    tab = np.zeros((2, 4, 128, 256), np.float64)
    dec = np.zeros((64, 2, 4, 2), np.float64)
    tt = np.arange(128)
    for d in range(2):
        p = (tt % 64) if d == 0 else 63 - (tt % 64)
        for h in range(4):
            g = lgd[d][h]
            A = (p + 1) * g
            Amid = 32 * g
            Alast = 64 * g
            cols = slice(h * 64, (h + 1) * 64)
            tab[d, 0, :, cols] = np.exp(A - Amid)[:, None]
            tab[d, 1, :, cols] = np.exp(Amid - A)[:, None]
            tab[d, 2, :, cols] = np.exp(A)[:, None]
            tab[d, 3, :, cols] = np.exp(Alast - A)[:, None]
            dec[:, d, h, :] = np.exp(Alast)
    c["ret_tab"] = np.ascontiguousarray(tab.transpose(2, 0, 1, 3).reshape(128, 8 * 256)).astype(np.float32)
    c["ret_dec"] = np.ascontiguousarray(dec.reshape(64, 16)).astype(np.float32)
    return c


def phase_gla(P, io, sc, l):
    m = P.mark()
    W = Ctx()
    W.sq = P.sbuf("sq", [128, 1024], F32)
    W.st = P.sbuf("st", [128, 64], F32)
    identf = P.sbuf("identf", [128, 128], F32)
    ident = P.sbuf("ident", [128, 128], BF16)
    P.dma("sp", identf[:], io.ident[:])
    P.copy(ident[:], identf[:])
    mask = P.sbuf("gmask", [128, 4, 128], F32)
    P.dma("sp", mask[:], io.gla_mask[:])
    gmm = P.sbuf("gmm", [128, 6, 128], F32)
    P.dma("sp", gmm[:], io.gla_mm[:])
    gci = P.sbuf("gci", [128, 2, 8], F32)
    P.dma("sp", gci[:], io.gla_ci[:])
    rtab = P.sbuf("rtab", [128, 8, 256], F32)
    P.dma("sp", rtab[:], io.ret_tab[:])
    rdec = P.sbuf("rdec", [64, 2, 4, 2], F32)
    P.dma("sp", rdec[:], io.ret_dec[:])
    gout = P.sbuf("gout", [128, 128], F32)
    P.dma("sp", gout[:], io.gains.ap(l * 768 + 640, [[0, 128], [1, 128]]))

    glt = [P.sbuf("glt", [128, 2304], F32) for _ in range(2)]
    vts = [P.sbuf("gvt", [128, 512], BF16) for _ in range(2)]
    oft = [P.sbuf("oft", [128, 512], F32) for _ in range(2)]
    E = P.sbuf("E", [128, 4, 256], F32)
    r1c = P.sbuf("r1c", [128, 256], F32)
    hdec = [P.sbuf("hdec", [64, 4, 8], F32) for _ in range(2)]
    qk = P.sbuf("qk", [128, 3, 256], BF16)
    khf = P.sbuf("khf", [128, 256], F32)
    khm = [[P.sbuf("khm", [128, NCH[mx], 256], BF16) for _ in range(2)] for mx in range(2)]
    TT = P.sbuf("TT", [64, 2, 4, 128], BF16)
    QhTm = [[P.sbuf("QhTm", [64, 4, NCH[mx], 128], BF16) for _ in range(2)] for mx in range(2)]
    for mx in range(2):
        for p_ in range(2):
            P.memset(QhTm[mx][p_][:], 0.0)
    Sm = [[P.sbuf("Sm", [128, 4, 128], BF16) for _ in range(2)] for mx in range(2)]
    S = [P.sbuf("S", [64, 4, 64], F32) for _ in range(2)]
    Sbf = [P.sbuf("Sbf", [64, 4, 64], BF16) for _ in range(8)]
    osum = P.sbuf("osum", [128, 256], F32)
    yt = [P.sbuf("yt", [128, 512], BF16) for _ in range(2)]
    psR = P.psum("psR", [128, 4, 256])
    psD = P.psum("psD", [64, 4, 8])
    psTQ = P.psum("psTQ", [64, 2, 4, 128], BF16)
    psTH = P.psum("psTH", [64, 4, 128], BF16)
    psDS = P.psum("psDS", [64, 8, 64])
    psSc = P.psum("psSc", [128, 4, 128])
    psO = P.psum("psO", [128, 256])

    def front(d, n_, i, mx):
        p_ = n_ % 2
        gl, vt, of = glt[p_], vts[p_], oft[p_]
        NC = NCH[mx]
        CS = 128 // NC
        if mx == 0:
            P.dma("sp", gl[:], sc.GL[i * 128:(i + 1) * 128, :])
            P.dma("sp", vt[:], sc.VV[i * 128:(i + 1) * 128, 512:1024])
            if d == 1:
                P.dma("sp", of[:], sc.OF[i * 128:(i + 1) * 128, :])
            q = gl[:, 0:256]
            k = gl[:, 256:512]
            Ev = lambda a: rtab[:, d * 4 + a, :]
        else:
            q = gl[:, 512:768]
            k = gl[:, 768 + d * 256:1024 + d * 256]
            lf0 = 1280 + d * 256
            for a in range(3):
                P.mm(psR[:, a, :], gmm[:, d * 3 + a, :], gl[:, lf0:lf0 + 256])
            P.ts(r1c[:], psR[:, 0, :], 43.0, -43.0, ALU.min, ALU.max)
            P.act(E[:, 0, :], r1c[:], AF.Exp)
            P.act(E[:, 1, :], r1c[:], AF.Exp, scale=-1.0)
            P.act(E[:, 2, :], psR[:, 1, :], AF.Exp)
            P.act(E[:, 3, :], psR[:, 2, :], AF.Exp)
            for h in range(4):
                P.mm(psD[:, h, :], gl[:, lf0 + h * 64:lf0 + (h + 1) * 64], gci[:, 1, :])
            P.act(hdec[p_][:], psD[:], AF.Exp)
            Ev = lambda a: E[:, a, :]
        P.tt(qk[:, 0, :], q, Ev(0), ALU.mult)
        P.tt(qk[:, 1, :], k, Ev(1), ALU.mult)
        P.tt(qk[:, 2, :], q, Ev(2), ALU.mult, eng="pool")
        P.tt(khf[:], k, Ev(3), ALU.mult, eng="pool")
        P.tt(khm[mx][p_][:], khf.ap(0, [[0, NC], [1, 256]]), gci.ap(mx * 8, [[1, NC], [0, 256]]), ALU.mult, eng="pool")
        for h in range(4):
            P.tr(psTQ[:, 0, h, :], qk[:, 0, h * 64:(h + 1) * 64], ident[:])
            P.tr(psTQ[:, 1, h, :], qk[:, 1, h * 64:(h + 1) * 64], ident[:])
            P.tr(psTH[:, h, :], qk[:, 2, h * 64:(h + 1) * 64], ident[:])
        P.copy(TT[:], psTQ[:], eng="act")
        P.copy(QhTm[mx][p_].ap(0, [[NC * 128, 4], [128 + CS, NC], [1, CS]]), psTH.ap(0, [[128, 4], [CS, NC], [1, CS]]), eng="dve")
        for h in range(4):
            P.mm(psSc[:, h, :], TT[:, 1, h, :], TT[:, 0, h, :])
        P.tt(Sm[mx][p_][:], psSc[:], mask.ap((mx * 2 + d) * 128, [[0, 4], [1, 128]]), ALU.mult)

    def back(d, n_, i, mx):
        p_ = n_ % 2
        gl, vt, of = glt[p_], vts[p_], oft[p_]
        NC = NCH[mx]
        corder = list(range(NC)) if d == 0 else list(range(NC - 1, -1, -1))
        if mx == 0:
            dec = lambda c: rdec.ap(d * 8 + c, [[2, 4], [0, 64]])
        else:
            dec = lambda c: hdec[p_].ap(c, [[8, 4], [0, 64]])
        vh = lambda h: vt[:, mx * 256 + h * 64:mx * 256 + (h + 1) * 64]
        St = S[mx]
        for n2, c in enumerate(corder):
            if n2 % 2 == 0:
                for c2 in corder[n2:n2 + 2]:
                    for h in range(4):
                        P.mm(psDS[:, (c2 % 2) * 4 + h, :], khm[mx][p_][:, c2, h * 64:(h + 1) * 64], vh(h))
            P.copy(Sbf[n2][:], St[:], eng="act")
            P.tt(St[:], St[:], dec(c), ALU.mult)
            P.tt(St[:], St[:], psDS[:, (c % 2) * 4:(c % 2) * 4 + 4, :], ALU.add)
        for h in range(4):
            oc = slice(h * 64, (h + 1) * 64)
            P.mm(psO[:, oc], Sm[mx][p_][:, h, :], vh(h), start=True, stop=False)
            for n2, c in enumerate(corder):
                P.mm(psO[:, oc], QhTm[mx][p_][:, h, c, :], Sbf[n2][:, h, :], start=False, stop=(n2 == NC - 1))
        if d == 0:
            P.copy(of[:, mx * 256:(mx + 1) * 256], psO[:], eng="act")
            if mx == 1:
                P.dma("sp", sc.OF[i * 128:(i + 1) * 128, :], of[:])
        else:
            ytt = yt[p_]
            P.tt(osum[:], psO[:], of[:, mx * 256:(mx + 1) * 256], ALU.add)
            rms_groups(P, W, osum.ap(0, [[64, 4], [1, 64]]), 4, 64, gout.ap(mx * 64, [[0, 4], [1, 64]]),
                       osum.ap(0, [[64, 4], [1, 64]]))
            P.tt(ytt[:, mx * 256:(mx + 1) * 256], osum[:], gl[:, 1792 + mx * 256:2048 + mx * 256], ALU.mult)
            if mx == 1:
                P.dma("sp", sc.Y[i * 128:(i + 1) * 128, :], ytt[:])

    for d in range(2):
        order = [32, 33] + list(range(32)) if d == 0 else [33, 32] + list(range(31, -1, -1))
        for mx in range(2):
            P.memset(S[mx][:], 0.0)
        units = [(d, n_, i, mx) for n_, i in enumerate(order) for mx in range(2)]
        front(*units[0])
        for u, un in enumerate(units):
            sb = P.record(lambda: back(*un))
            sf = P.record(lambda: front(*units[u + 1])) if u + 1 < len(units) else []
            P.play(sb, sf)
    P.release(m)


NSLOT = NEXP * CAP


def moe_consts():
    c = {}
    s = np.arange(128)[:, None]
    t = np.arange(128)[None, :]
    c["moe_U"] = (s < t).astype(np.float32)
    c["moe_eoff"] = np.broadcast_to((np.arange(NEXP) * CAP).astype(np.float32), (128, NEXP)).copy()
    return c


def phase_out(P, io, sc, l, RT):
    m = P.mark()
    modL = ModSlice(P, sc, 0, 2048, 5120)
    modC = ModSlice(P, sc, 1, 2048, 5120)
    W = Ctx()
    W.sq = P.sbuf("sq", [128, 1024], F32)
    W.st = P.sbuf("st", [128, 64], F32)
    identf = P.sbuf("identf", [128, 128], F32)
    ident = P.sbuf("ident", [128, 128], BF16)
    P.dma("sp", identf[:], io.ident[:])
    P.copy(ident[:], identf[:])
    wout = P.sbuf("wout", [128, 8, D], BF16)
    for k in range(8):
        P.dma("pool", wout[:, k, :], io.w_out.ap(l * D * D + k * 128 * D, [[D, 128], [1, D]]))
    wr = P.sbuf("wr", [128, 8, 36], F32)
    P.dma("sp", wr[:], io.moe_wr.ap(l * D * 36, [[36, 128], [128 * 36, 8], [1, 36]]))
    br = P.sbuf("br", [128, 36], F32)
    P.dma("sp", br[:], io.moe_br.ap(l * 36, [[0, 128], [1, 36]]))
    Uf = P.sbuf("Uf", [128, 128], F32)
    U = P.sbuf("U", [128, 128], BF16)
    P.dma("sp", Uf[:], io.moe_U[:])
    P.copy(U[:], Uf[:])
    onesb = P.sbuf("onesb", [128, 128], BF16)
    P.memset(onesb[:], 1.0)
    eoff = P.sbuf("eoff", [128, NEXP], F32)
    P.dma("sp", eoff[:], io.moe_eoff[:])
    cnt = P.sbuf("cnt", [128, NEXP], F32)
    P.memset(cnt[:], 0.0)

    yts = [P.sbuf("ytok", [128, 512], BF16) for _ in range(2)]
    yT = [P.sbuf("yT", [128, 8, 128], BF16) for _ in range(2)]
    xt = [P.sbuf("xo", [128, D], F32) for _ in range(2)]
    hfs = [P.sbuf("hf", [128, D], F32) for _ in range(2)]
    Wr = Ctx()
    Wr.st = P.sbuf("str", [128, 64], F32)
    hb = [P.sbuf("hb", [128, D], BF16) for _ in range(2)]
    hT = P.sbuf("hT", [128, 8, 128], F32)
    lg = P.sbuf("lg", [128, 36], F32)
    r = P.sbuf("rw", [128, 12, 32], F32)
    selb = P.sbuf("selb", [128, NEXP], BF16)
    psT = P.psum("psT", [128, 4, 128], BF16)
    psA = [P.psum("psA", [128, 512]) for _ in range(2)]
    psH = [P.psum("psH", [128, 4, 128]) for _ in range(2)]
    psL = P.psum("psL", [128, 36])
    psP = P.psum("psPos", [128, NEXP])

    def stageA(i):
        hf = hfs[i % 2]
        isctx = i >= 32
        mod = modC if isctx else modL
        yt, yTt, x, hbt = yts[i % 2], yT[i % 2], xt[i % 2], hb[i % 2]
        P.dma("sp", yt[:], sc.Y[i * 128:(i + 1) * 128, :])
        P.dma("sp", yTt[:, 0:4, :], sc.YT.ap(i * 128, [[T, 128], [128 * T, 4], [1, 128]]))
        P.dma("sp", x[:], sc.xres[i * 128:(i + 1) * 128, :])
        for c in range(4):
            P.tr(psT[:, c, :], yt[:, c * 128:(c + 1) * 128], ident[:])
        P.copy(yTt[:, 4:8, :], psT[:], eng="act")
        for half in range(2):
            ps = psA[half]
            for c in range(8):
                P.mm(ps[:], yTt[:, c, :], wout[:, c, half * 512:(half + 1) * 512], start=(c == 0), stop=(c == 7))
            hs = slice(half * 512, (half + 1) * 512)
            P.tt(W.sq[:, hs], ps[:], mod[:, 2048 + half * 512:2048 + (half + 1) * 512], ALU.mult)
            P.tt(x[:, hs], x[:, hs], W.sq[:, hs], ALU.add)
        P.dma("sp", sc.xres[i * 128:(i + 1) * 128, :], x[:])
        ssx = W.st[:, 48:49]
        P.act(W.sq[:], x[:], AF.Square, accum=ssx)
        P.ts(ssx, ssx, 1.0 / D, EPS, ALU.mult, ALU.add)
        P.act(ssx, ssx, AF.Sqrt)
        P.recip(ssx, ssx)
        P.stt(W.sq[:], x[:], ssx, mod[:, 4096:5120], ALU.mult, ALU.mult)
        P.tt(hf[:], W.sq[:], mod[:, 3072:4096], ALU.add)
        P.copy(hbt[:], hf[:], eng="act")

    def stageB(i):
        hf = hfs[i % 2]
        hbt = hb[i % 2]
        for rnd in range(2):
            ph = psH[rnd]
            for k in range(4):
                P.tr(ph[:, k, :], hf[:, (rnd * 4 + k) * 128:(rnd * 4 + k + 1) * 128], identf[:])
            P.copy(hT[:, rnd * 4:(rnd + 1) * 4, :], ph[:], eng=("act" if rnd == 0 else "dve"))
        for k in range(8):
            P.mm(psL[:], hT[:, k, :], wr[:, k, :], start=(k == 0), stop=(k == 7))
        P.tt(lg[:], psL[:], br[:], ALU.add)
        route_tile(P, Wr, i, lg, r, selb, U, onesb, eoff, cnt, psP, RT)
        for a in range(2):
            idx = RT.slot[:, i, a:a + 1]
            P.dma("pool", sc.XS[:, :], hbt[:], extra_reads=[idx],
                  indirect=lambda e, idx=idx, hbt=hbt: e.indirect_dma_start(
                      out=sc.XS.t[:, :], out_offset=bass.IndirectOffsetOnAxis(ap=idx.ap, axis=0),
                      in_=hbt.t[:, :], in_offset=None, bounds_check=P.breg, oob_is_err=False))

    stageA(0)
    for i in range(NT):
        sB = P.record(lambda: stageB(i))
        sA = P.record(lambda: stageA(i + 1)) if i + 1 < NT else []
        P.play(sB, sA)
    P.release(m)


def route_tile(P, W, i, lg, r, selb, U, onesb, eoff, cnt, psP, RT):
    BIG = 1.0e4
    g4 = lg[:, 0:4]
    f32v = lg[:, 4:36]
    gmax = W.st[:, 0:1]
    ngmax = W.st[:, 1:2]
    sg = W.st[:, 2:3]
    m1 = W.st[:, 3:4]
    m2 = W.st[:, 4:5]
    nm1 = W.st[:, 5:6]
    se = W.st[:, 6:7]
    P.red(gmax, g4, op=ALU.max)
    P.ts(ngmax, gmax, -1.0, None, ALU.mult)
    oh = r[:, 0, 0:4]
    P.ts(oh, g4, gmax, None, ALU.is_equal)
    eg = r[:, 0, 8:12]
    P.act(eg, g4, AF.Exp, bias=ngmax, accum=sg)
    pen = r[:, 0, 16:20]
    P.ts(pen, oh, BIG, -BIG, ALU.mult, ALU.add)
    lfm = r[:, 1, :]
    P.tt(r.ap(32, [[8, 4], [1, 8]]), lg.ap(4, [[8, 4], [1, 8]]), r.ap(16, [[1, 4], [0, 8]]), ALU.add)
    P.red(m1, lfm, op=ALU.max)
    eq1 = r[:, 2, :]
    P.ts(eq1, lfm, m1, None, ALU.is_equal)
    lfm2 = r[:, 3, :]
    P.stt(lfm2, eq1, -BIG, lfm, ALU.mult, ALU.add)
    P.red(m2, lfm2, op=ALU.max)
    sel = r[:, 4, :]
    P.ts(sel, lfm, m2, None, ALU.is_ge)
    eq2 = r[:, 5, :]
    P.tt(eq2, sel, eq1, ALU.subtract)
    P.ts(nm1, m1, -1.0, None, ALU.mult)
    ee = r[:, 6, :]
    P.act(ee, lfm, AF.Exp, bias=nm1)
    P.tt(ee, ee, sel, ALU.mult)
    P.red(se, ee)
    P.tt(se, se, sg, ALU.mult)
    P.recip(se, se)
    P.ts(ee, ee, se, None, ALU.mult)
    P.copy(selb[:], sel)
    P.mm(psP[:], U[:], selb[:], start=True, stop=True)
    pos = r[:, 7, :]
    P.tt(pos, psP[:], cnt[:], ALU.add)
    P.mm(psP[:], onesb[:], selb[:], start=True, stop=True)
    P.tt(cnt[:], cnt[:], psP[:], ALU.add)
    ok = r[:, 8, :]
    P.ts(ok, pos, float(CAP), None, ALU.is_lt)
    sv = r[:, 9, :]
    P.tt(sv, pos, eoff[:], ALU.add)
    P.tt(sv, sv, ok, ALU.mult)
    P.ts(ok, ok, -float(NSLOT + 8), float(NSLOT + 8), ALU.mult, ALU.add)
    P.tt(sv, sv, ok, ALU.add)
    tmp = r[:, 10, :]
    sf = W.st[:, 8:10]
    for a, eq in enumerate((eq1, eq2)):
        P.tt(tmp, sv, eq, ALU.mult)
        P.red(W.st[:, 8 + a:9 + a], tmp)
        P.tt(tmp, ee, eq, ALU.mult)
        P.red(RT.gate[:, i, a:a + 1], tmp)
    P.ts(W.st[:, 10:12], sf, float(NSLOT), None, ALU.is_lt)
    P.tt(RT.gate[:, i, :], RT.gate[:, i, :], W.st[:, 10:12], ALU.mult)
    P.copy(RT.slot[:, i, :], sf)


def phase_experts(P, io, sc, l):
    m = P.mark()
    identf = P.sbuf("identf", [128, 128], F32)
    ident = P.sbuf("ident", [128, 128], BF16)
    P.dma("sp", identf[:], io.ident[:])
    P.copy(ident[:], identf[:])
    w1 = [P.sbuf("w1", [128, 8, FF], BF16) for _ in range(2)]
    w3 = [P.sbuf("w3", [128, 8, FF], BF16) for _ in range(2)]
    w2 = [P.sbuf("w2", [128, 4, D], BF16) for _ in range(2)]
    xs = [P.sbuf("xs", [128, D], BF16) for _ in range(3)]
    xT = [P.sbuf("xT", [128, 8, 512], BF16) for _ in range(2)]
    hT = [P.sbuf("hTe", [128, 4, 512], BF16) for _ in range(2)]
    s1 = [P.sbuf("s1", [128, 512], F32) for _ in range(2)]
    ys = [P.sbuf("ys", [128, D], BF16) for _ in range(2)]
    psT = [P.psum("psT", [128, 8, 128], BF16) for _ in range(2)]
    ps1 = [P.psum("ps1", [128, 512]) for _ in range(2)]
    ps3 = [P.psum("ps3", [128, 512]) for _ in range(2)]
    psY = [P.psum("psY", [128, 512]) for _ in range(2)]
    cnt = {"nx": 0, "ny": 0}
    groups = [(e, g) for e in range(NEXP) for g in range(CAP // 512)]

    def load_w(e):
        a, b3, c2 = w1[e % 2], w3[e % 2], w2[e % 2]
        base = (l * NEXP + e)
        for k in range(0, 8, 2):
            P.dma("pool", a[:, k:k + 2, :], io.moe_w1.ap(base * D * FF + k * 128 * FF, [[FF, 128], [128 * FF, 2], [1, FF]]))
            P.dma("pool", b3[:, k:k + 2, :], io.moe_w3.ap(base * D * FF + k * 128 * FF, [[FF, 128], [128 * FF, 2], [1, FF]]))
        for k in range(4):
            P.dma("pool", c2[:, k, :], io.moe_w2.ap(base * FF * D + k * 128 * D, [[D, 128], [1, D]]))

    def prep(gi):
        e, g = groups[gi]
        if g == 0:
            load_w(e)
        xTt = xT[gi % 2]
        for t4 in range(4):
            row0 = e * CAP + g * 512 + t4 * 128
            xst = xs[cnt["nx"] % 3]
            pT = psT[cnt["nx"] % 2]
            cnt["nx"] += 1
            P.dma("sp", xst[:], sc.XS[row0:row0 + 128, :])
            for k in range(8):
                P.tr(pT[:, k, :], xst[:, k * 128:(k + 1) * 128], ident[:])
            P.copy(xTt[:, 0:4, t4 * 128:(t4 + 1) * 128], pT[:, 0:4, :], eng="act")
            P.copy(xTt[:, 4:8, t4 * 128:(t4 + 1) * 128], pT[:, 4:8, :], eng="dve")

    def compute(gi):
        e, g = groups[gi]
        a, b3, c2 = w1[e % 2], w3[e % 2], w2[e % 2]
        xTt = xT[gi % 2]
        hTt = hT[gi % 2]
        for fc in range(4):
            p1, p3, s = ps1[fc % 2], ps3[fc % 2], s1[fc % 2]
            for k in range(8):
                P.mm(p1[:], a[:, k, fc * 128:(fc + 1) * 128], xTt[:, k, :], start=(k == 0), stop=(k == 7))
            for k in range(8):
                P.mm(p3[:], b3[:, k, fc * 128:(fc + 1) * 128], xTt[:, k, :], start=(k == 0), stop=(k == 7))
            P.act(s[:], p1[:], AF.Silu)
            P.tt(hTt[:, fc, :], s[:], p3[:], ALU.mult)
        for t4 in range(4):
            row0 = e * CAP + g * 512 + t4 * 128
            yst = ys[cnt["ny"] % 2]
            cnt["ny"] += 1
            for half in range(2):
                py = psY[half]
                for fc in range(4):
                    P.mm(py[:], hTt[:, fc, t4 * 128:(t4 + 1) * 128], c2[:, fc, half * 512:(half + 1) * 512],
                         start=(fc == 0), stop=(fc == 3))
                P.copy(yst[:, half * 512:(half + 1) * 512], py[:], eng=("act" if half == 0 else "dve"))
            P.dma("sp", sc.YS[row0:row0 + 128, :], yst[:])

    prep(0)
    for gi in range(len(groups)):
        sb = P.record(lambda: compute(gi))
        sa = P.record(lambda: prep(gi + 1)) if gi + 1 < len(groups) else []
        P.play(sb, sa)
    P.release(m)


def phase_combine(P, io, sc, l, RT, out):
    m = P.mark()
    modL = ModSlice(P, sc, 0, 5120, 6144)
    modC = ModSlice(P, sc, 1, 5120, 6144)
    W = Ctx()
    ya = [P.sbuf("ya", [128, D], BF16) for _ in range(2)]
    yb = [P.sbuf("yb", [128, D], BF16) for _ in range(2)]
    xt = [P.sbuf("xc", [128, D], F32) for _ in range(2)]
    t1 = P.sbuf("t1", [128, D], F32)
    for bufs in (ya, yb):
        for b in bufs:
            P.memset(b[:], 0.0)
    last = l == DEPTH - 1
    for i in range(NT):
        if last and i >= 32:
            break
        mod = modC if i >= 32 else modL
        yat, ybt, x = ya[i % 2], yb[i % 2], xt[i % 2]
        P.dma("sp", x[:], sc.xres[i * 128:(i + 1) * 128, :])
        for a, yt in enumerate((yat, ybt)):
            idx = RT.slot[:, i, a:a + 1]
            P.dma("pool", yt[:], sc.YS[:, :], extra_reads=[idx],
                  indirect=lambda e, idx=idx, yt=yt: e.indirect_dma_start(
                      out=yt.t[:, :], out_offset=None, in_=sc.YS.t[:, :],
                      in_offset=bass.IndirectOffsetOnAxis(ap=idx.ap, axis=0),
                      bounds_check=P.breg, oob_is_err=False))
        P.ts(t1[:], yat[:], RT.gate[:, i, 0:1], None, ALU.mult)
        P.stt(t1[:], ybt[:], RT.gate[:, i, 1:2], t1[:], ALU.mult, ALU.add)
        P.tt(t1[:], t1[:], mod[:, 5120:6144], ALU.mult)
        P.tt(x[:], x[:], t1[:], ALU.add)
        if last:
            P.dma("sp", out[i * 128:(i + 1) * 128, :], x[:])
        else:
            P.dma("sp", sc.xres[i * 128:(i + 1) * 128, :], x[:])
    P.release(m)


def build_program(nlayers=DEPTH, dbg_x=False):
    nc = bass.Bass("TRN2", target_bir_lowering=False)
    P = Prog(nc)
    io = declare_io(P, None)
    sc = alloc_scratch(P)
    out = P.dram("out", [NLAT, D], F32, kind="ExternalOutput")
    dbg = P.dram("dbg_xres", [T, D], F32, kind="ExternalOutput") if dbg_x else None
    lbt = P.sbuf("lbt", [128, 4, 512], F32)
    RT = Ctx()
    RT.slot = P.sbuf("rt_slot", [128, NT, 2], I32)
    RT.gate = P.sbuf("rt_gate", [128, NT, 2], F32)
    P.breg = nc.gpsimd.to_reg(NSLOT - 1)
    phase_lb(P, io, lbt)
    for l in range(nlayers):
        phase_mod(P, io, sc, l)
        phase_proj(P, io, sc, l, lbt)
        phase_mla(P, io, sc)
        phase_na(P, io, sc, l)
        phase_gla(P, io, sc, l)
        phase_out(P, io, sc, l, RT)
        phase_experts(P, io, sc, l)
        phase_combine(P, io, sc, l if nlayers == DEPTH else -1, RT, out)
    if dbg_x:
        m = P.mark()
        xb = [P.sbuf("dbgx", [128, D], F32) for _ in range(2)]
        for i in range(NT):
            P.dma("sp", xb[i % 2][:], sc.xres[i * 128:(i + 1) * 128, :])
            P.dma("sp", dbg[i * 128:(i + 1) * 128, :], xb[i % 2][:])
        P.release(m)
    P.barrier()
    P.stats = (P.nins, P.nwaits, P.nsem)
    P.close()
    return nc


def make_in_maps(inputs):
    c = _consts()
    c.update(gla_consts())
    c.update(moe_consts())
    f = lambda a: np.ascontiguousarray(np.asarray(a, dtype=np.float32))
    gains = np.concatenate([inputs[k] for k in ["mla_g_cq", "mla_g_ckv", "mla_g_qn", "mla_g_qr", "mla_g_kn", "mla_g_kr",
                                                "na_g_q", "na_g_k", "ret_g_out", "hg_g_out"]], axis=1)
    shared = {
        "w_ada": f(inputs["w_ada"]), "b_ada": f(inputs["b_ada"]), "w_in": f(inputs["w_in"]), "w_out": f(inputs["w_out"]),
        "gains": f(gains), "mla_w_uq": f(inputs["mla_w_uq"]), "mla_w_ukv": f(inputs["mla_w_ukv"]),
        "ident": c["ident"], "rope": c["rope"], "hg_lb_raw": f(inputs["hg_lb_raw"]),
        "na_bias": na_bias_tables(np.asarray(inputs["na_rpb"], dtype=np.float32)),
        "gla_mask": c["gla_mask"], "gla_mm": c["gla_mm"], "gla_ci": c["gla_ci"], "ret_tab": c["ret_tab"], "ret_dec": c["ret_dec"],
        "moe_wr": f(np.concatenate([inputs["moe_w_rg"], inputs["moe_w_re"]], axis=2)),
        "moe_br": f(np.concatenate([inputs["moe_b_rg"], inputs["moe_b_re"]], axis=1)),
        "moe_U": c["moe_U"], "moe_eoff": c["moe_eoff"],
        "moe_w1": f(inputs["moe_w1"]), "moe_w3": f(inputs["moe_w3"]), "moe_w2": f(inputs["moe_w2"]),
    }
    maps = []
    for b in range(8):
        cv = np.stack([np.asarray(inputs["c"][b], np.float32), np.asarray(inputs["c_ctx"], np.float32)], axis=0)
        d = dict(shared)
        d["x"] = f(inputs["x"][b])
        d["ctx"] = f(inputs["ctx"][b])
        d["cvT"] = np.ascontiguousarray(cv.reshape(2, 8, 128).transpose(2, 1, 0))
        maps.append(d)
    return maps


def kernel(**inputs):
    nc = build_program()
    maps = make_in_maps(inputs)
    res = run_bass_kernel_spmd(nc, maps, core_ids=list(range(8)))
    return np.stack([np.asarray(r["out"], dtype=np.float32) for r in res.results], axis=0)
```

```python
import numpy as np
import concourse.bass as bass
import concourse.mybir as mybir
from concourse.alu_op_type import AluOpType as ALU
from concourse.bass_utils import run_bass_kernel_spmd

F32 = mybir.dt.float32
BF16 = mybir.dt.bfloat16
I32 = mybir.dt.int32
U32 = mybir.dt.uint32
AF = mybir.ActivationFunctionType
AX = mybir.AxisListType

D = 1024
NLAT = 4096
NCTX = 256
T = NLAT + NCTX
NT = T // 128
DEPTH = 4
PTOT = 3424
EPS = 1e-6
MLA_SCALE = 96.0 ** -0.5
NA_SCALE = 0.125
NEG = -30000.0
CAP = 1024
NEXP = 32
FF = 512
PROJ_INTERLEAVE = True


class View:
    __slots__ = ("buf", "ap")

    def __init__(self, buf, ap):
        self.buf = buf
        self.ap = ap


class Buf:
    def __init__(self, name, t, kind, shape, dt):
        self.name = name
        self.t = t
        self.kind = kind
        self.shape = list(shape)
        self.dt = dt
        self.w = {}
        self.r = {}
        self.dsem = None
        self.dcount = 0
        self.dkey = None
        self.F = int(np.prod(shape[1:]))

    def __getitem__(self, idx):
        return View(self, self.t[idx])

    def ap(self, off, dims, p0=0, npart=None):
        if npart is None:
            npart = self.shape[0] - p0
        if self.kind == "dram":
            return View(self, bass.AP(self.t, off, dims))
        return View(self, bass.AP(self.t, p0 * self.F + off, [[self.F, npart]] + [list(d) for d in dims]))


class Prog:
    def __init__(self, nc):
        self.nc = nc
        self.engs = {"pe": nc.tensor, "dve": nc.vector, "act": nc.scalar, "pool": nc.gpsimd, "sp": nc.sync}
        self.sem = {}
        self.cnt = {}
        self.waited = {k: {} for k in self.engs}
        self._stack = []
        self._semcms = []
        self.free_dsems = []
        self.nsem = 0
        self.dsems = []
        self.nwaits = 0
        self.nins = 0
        self.uid = 0
        self.rec = None
        for k in self.engs:
            self.sem[k] = self._newsem("e_" + k)
            self.cnt[k] = 0

    def _newsem(self, name):
        cm = self.nc.semaphore(name)
        s = cm.__enter__()
        self._semcms.append(cm)
        self.nsem += 1
        return s

    def mark(self):
        return len(self._stack)

    def release(self, mark):
        self.barrier()
        while len(self._stack) > mark:
            cm, buf = self._stack.pop()
            cm.__exit__(None, None, None)
            if buf is not None and buf.dsem is not None:
                self.free_dsems.append((buf.dsem, buf.dcount, buf.dkey))
                self.dsems.remove(buf)
                buf.dsem = None

    def sbuf(self, name, shape, dt):
        self.uid += 1
        name = "%s_%d" % (name, self.uid)
        cm = self.nc.sbuf_tensor(name, list(shape), dt)
        t = cm.__enter__()
        b = Buf(name, t, "sbuf", shape, dt)
        self._stack.append((cm, b))
        return b

    def psum(self, name, shape, dt=F32):
        self.uid += 1
        name = "%s_%d" % (name, self.uid)
        cm = self.nc.psum_tensor(name, list(shape), dt)
        t = cm.__enter__()
        b = Buf(name, t, "psum", shape, dt)
        self._stack.append((cm, b))
        return b

    def dram(self, name, shape, dt, kind="Internal"):
        t = self.nc.dram_tensor(name, list(shape), dt, kind=kind)
        return Buf(name, t, "dram", shape, dt)

    def _wait(self, eng, key, tok):
        sem, val = tok
        w = self.waited[eng]
        if w.get(key, 0) >= val:
            return
        self.engs[eng].wait_ge(sem, val)
        w[key] = val
        self.nwaits += 1

    def _deps(self, eng, reads, writes, pe_accum=False):
        for b in reads:
            for k, t in b.w.items():
                self._wait(eng, k, t)
        for b in writes:
            for k, t in b.w.items():
                if pe_accum and k == "e_pe":
                    continue
                self._wait(eng, k, t)
            for k, t in b.r.items():
                self._wait(eng, k, t)

    def record(self, fn):
        assert self.rec is None
        self.rec = []
        fn()
        r = self.rec
        self.rec = None
        return r

    def play(self, *streams):
        streams = [st for st in streams if st]
        pos = [0] * len(streams)
        total = max(len(st) for st in streams) if streams else 0
        for step in range(1, total + 1):
            for k, st in enumerate(streams):
                tgt = (len(st) * step + total - 1) // total
                while pos[k] < tgt:
                    st[pos[k]]()
                    pos[k] += 1

    def op(self, eng, fn, reads=(), writes=(), pe_accum=False):
        if self.rec is not None:
            self.rec.append(lambda: self._op(eng, fn, reads, writes, pe_accum))
            return None
        return self._op(eng, fn, reads, writes, pe_accum)

    def _op(self, eng, fn, reads=(), writes=(), pe_accum=False):
        reads = [v.buf for v in reads if isinstance(v, View)]
        writes = [v.buf for v in writes if isinstance(v, View)]
        self._deps(eng, reads, writes, pe_accum)
        ins = fn(self.engs[eng])
        self.cnt[eng] += 1
        self.nins += 1
        ins.then_inc(self.sem[eng], 1)
        key = "e_" + eng
        tok = (self.sem[eng], self.cnt[eng])
        for b in reads:
            b.r[key] = tok
        for b in writes:
            b.w[key] = tok
        return ins

    def dma(self, q, out, in_, extra_reads=(), indirect=None, **kw):
        if self.rec is not None:
            self.rec.append(lambda: self._dma(q, out, in_, extra_reads, indirect, **kw))
            return None
        return self._dma(q, out, in_, extra_reads, indirect, **kw)

    def _dma(self, q, out, in_, extra_reads=(), indirect=None, **kw):
        dst, src = out.buf, in_.buf
        sb = dst if dst.kind == "sbuf" else src
        assert sb.kind == "sbuf"
        if sb.dsem is None:
            if self.free_dsems:
                sb.dsem, sb.dcount, sb.dkey = self.free_dsems.pop()
            else:
                sb.dkey = "ds%d" % self.nsem
                sb.dsem = self._newsem(sb.dkey)
                sb.dcount = 0
            self.dsems.append(sb)
        rd = [src] + [v.buf for v in extra_reads]
        self._deps(q, rd, [dst])
        if indirect is None:
            ins = self.engs[q].dma_start(out=out.ap, in_=in_.ap, **kw)
        else:
            ins = indirect(self.engs[q])
        sb.dcount += 16
        self.nins += 1
        ins.then_inc(sb.dsem, 16)
        key = sb.dkey
        tok = (sb.dsem, sb.dcount)
        for b in rd:
            b.r[key] = tok
        dst.w[key] = tok
        return ins

    def barrier(self):
        for eng in self.engs:
            for k in self.engs:
                if self.cnt[k] and k != eng:
                    self._wait(eng, "e_" + k, (self.sem[k], self.cnt[k]))
            for sb in self.dsems:
                if sb.dcount:
                    self._wait(eng, sb.dkey, (sb.dsem, sb.dcount))

    def close(self):
        while self._stack:
            self._stack.pop()[0].__exit__(None, None, None)
        while self._semcms:
            self._semcms.pop().__exit__(None, None, None)

    def mm(self, out, lhsT, rhs, start=True, stop=True):
        return self.op("pe", lambda e: e.matmul(out.ap, lhsT.ap, rhs.ap, start=start, stop=stop),
                       [lhsT, rhs], [out], pe_accum=True)

    def tr(self, out, in_, ident):
        return self.op("pe", lambda e: e.transpose(out.ap, in_.ap, ident.ap), [in_, ident], [out], pe_accum=True)

    def act(self, out, in_, func, bias=None, scale=None, accum=None, eng="act"):
        kw = {}
        rd = [in_]
        wr = [out]
        if bias is not None:
            kw["bias"] = bias.ap if isinstance(bias, View) else bias
            rd.append(bias)
        if scale is not None:
            kw["scale"] = scale.ap if isinstance(scale, View) else scale
            rd.append(scale)
        if accum is not None:
            kw["accum_out"] = accum.ap
            wr.append(accum)
        return self.op("act", lambda e: e.activation(out.ap, in_.ap, func, **kw), rd, wr)

    def tt(self, out, a, b, op, eng="dve"):
        return self.op(eng, lambda e: e.tensor_tensor(out.ap, a.ap, b.ap, op), [a, b], [out])

    def ts(self, out, a, s1, s2, op0, op1=None, eng="dve"):
        rd = [a, s1, s2]
        x1 = s1.ap if isinstance(s1, View) else s1
        x2 = s2.ap if isinstance(s2, View) else s2
        if op1 is None:
            return self.op(eng, lambda e: e.tensor_scalar(out.ap, a.ap, x1, None, op0), rd, [out])
        return self.op(eng, lambda e: e.tensor_scalar(out.ap, a.ap, x1, x2, op0, op1), rd, [out])

    def stt(self, out, a, s, b, op0, op1):
        x = s.ap if isinstance(s, View) else s
        return self.op("dve", lambda e: e.scalar_tensor_tensor(out.ap, a.ap, x, b.ap, op0, op1), [a, s, b], [out])

    def red(self, out, a, op=ALU.add, axis=AX.X):
        return self.op("dve", lambda e: e.tensor_reduce(out.ap, a.ap, axis, op), [a], [out])

    def recip(self, out, a):
        return self.op("dve", lambda e: e.reciprocal(out.ap, a.ap), [a], [out])

    def copy(self, out, a, eng="dve"):
        if eng == "act":
            return self.op("act", lambda e: e.copy(out.ap, a.ap), [a], [out])
        return self.op(eng, lambda e: e.tensor_copy(out.ap, a.ap), [a], [out])

    def memset(self, out, val, eng="dve"):
        return self.op(eng, lambda e: e.memset(out.ap, val), [], [out])


def _rope_tables():
    pos = np.arange(NLAT)
    rows = (pos // 64).astype(np.float32)
    cols = (pos % 64).astype(np.float32)
    inv = (10000.0 ** (-np.arange(0, 16, 2, dtype=np.float32) / 16)).astype(np.float32)
    ang = np.concatenate([rows[:, None] * inv, cols[:, None] * inv], axis=-1).astype(np.float32)
    c, s = np.cos(ang).astype(np.float32), np.sin(ang).astype(np.float32)
    c1, c2, s1, s2 = c[:, :8], c[:, 8:], s[:, :8], s[:, 8:]
    C32 = np.concatenate([c1, c1, c2, c2], axis=1)
    S32 = np.concatenate([-s1, s1, -s2, s2], axis=1)
    C32 = np.concatenate([C32, np.ones((NCTX, 32), np.float32)], axis=0)
    S32 = np.concatenate([S32, np.zeros((NCTX, 32), np.float32)], axis=0)
    return np.ascontiguousarray(np.concatenate([C32, S32], axis=1).astype(np.float32))


def _consts():
    c = {}
    c["ident"] = np.eye(128, dtype=np.float32)
    c["rope"] = _rope_tables()
    return c


class Ctx:
    pass


def declare_io(P, g):
    io = Ctx()
    ext = lambda n, s: P.dram(n, s, F32, kind="ExternalInput")
    io.x = ext("x", [NLAT, D])
    io.ctx = ext("ctx", [NCTX, D])
    io.cvT = ext("cvT", [128, 8, 2])
    io.w_ada = ext("w_ada", [DEPTH, D, 6 * D])
    io.b_ada = ext("b_ada", [DEPTH, 6 * D])
    io.w_in = ext("w_in", [DEPTH, D, PTOT])
    io.w_out = ext("w_out", [DEPTH, D, D])
    io.gains = ext("gains", [DEPTH, 768])
    io.w_uq = ext("mla_w_uq", [DEPTH, 192, 384])
    io.w_ukv = ext("mla_w_ukv", [DEPTH, 128, 512])
    io.ident = ext("ident", [128, 128])
    io.rope = ext("rope", [T, 64])
    io.hg_lb_raw = ext("hg_lb_raw", [DEPTH, 256])
    io.na_bias = ext("na_bias", [DEPTH, 128, 100, 128])
    io.gla_mask = ext("gla_mask", [128, 512])
    io.gla_mm = ext("gla_mm", [128, 768])
    io.gla_ci = ext("gla_ci", [128, 16])
    io.ret_tab = ext("ret_tab", [128, 2048])
    io.ret_dec = ext("ret_dec", [64, 16])
    io.moe_wr = ext("moe_wr", [DEPTH, D, 36])
    io.moe_br = ext("moe_br", [DEPTH, 36])
    io.moe_U = ext("moe_U", [128, 128])
    io.moe_eoff = ext("moe_eoff", [128, NEXP])
    io.moe_w1 = ext("moe_w1", [DEPTH, NEXP, D, FF])
    io.moe_w3 = ext("moe_w3", [DEPTH, NEXP, D, FF])
    io.moe_w2 = ext("moe_w2", [DEPTH, NEXP, FF, D])
    return io


def alloc_scratch(P):
    s = Ctx()
    s.xres = P.dram("xres", [T, D], F32)
    s.mlaQT = P.dram("mlaQT", [4, 96, T], BF16)
    s.mlaKT = P.dram("mlaKT", [4, 96, T], BF16)
    s.naQT = P.dram("naQT", [256, T], BF16)
    s.naKT = P.dram("naKT", [256, T], BF16)
    s.VV = P.dram("VV", [T, 1024], BF16)
    s.GL = P.dram("GL", [T, 2304], F32)
    s.YT = P.dram("YT", [512, T], BF16)
    s.OF = P.dram("OF", [T, 512], F32)
    s.MOD = P.dram("MOD", [2, 6 * D], F32)
    s.XS = P.dram("XS", [NEXP * CAP, D], BF16)
    s.YS = P.dram("YS", [NEXP * CAP, D], BF16)
    s.Y = P.dram("Y", [T, 512], BF16)
    return s


def rms_groups(P, W, src, G, n, gain, out, scale=1.0):
    pool = getattr(W, "pool", None)
    if pool:
        W.pi = (getattr(W, "pi", 0) + 1) % len(pool)
        Wsq, Wst = pool[W.pi]
    else:
        Wsq, Wst = W.sq, W.st
    sq = Wsq.ap(0, [[n, G], [1, n]])
    P.act(sq, src, AF.Square)
    ss = Wst.ap(0, [[1, G]])
    P.red(ss, sq)
    ms = Wst.ap(16, [[1, G]])
    P.ts(ms, ss, 1.0 / n, EPS, ALU.mult, ALU.add)
    P.act(ms, ms, AF.Sqrt)
    rs = Wst.ap(32, [[1, G]])
    P.recip(rs, ms)
    if scale != 1.0:
        P.ts(rs, rs, float(scale), None, ALU.mult)
    rsb = Wst.ap(32, [[1, G], [0, n]])
    P.tt(sq, src, rsb, ALU.mult)
    P.tt(out, sq, gain, ALU.mult)


class ModSlice:
    def __init__(self, P, sc, v, c0, c1):
        self.c0 = c0
        self.buf = P.sbuf("mod", [128, c1 - c0], F32)
        P.dma("sp", self.buf[:], sc.MOD.ap(v * 6 * D + c0, [[0, 128], [1, c1 - c0]]))

    def __getitem__(self, idx):
        rows, cols = idx
        return self.buf[rows, cols.start - self.c0:cols.stop - self.c0]


def phase_mod(P, io, sc, l):
    m = P.mark()
    cv = P.sbuf("cv", [128, 8, 2], F32)
    cvs = P.sbuf("cvs", [128, 8, 2], BF16)
    P.dma("sp", cv[:], io.cvT[:])
    P.act(cvs[:], cv[:], AF.Silu)
    wb = [P.sbuf("wada", [128, 8, 512], BF16) for _ in range(2)]
    bb = [P.sbuf("bada", [128, 512], F32) for _ in range(2)]
    mt = [P.sbuf("modt", [128, 512], F32) for _ in range(4)]
    ps = [P.psum("pmod", [128, 512]) for _ in range(2)]
    for g in range(12):
        w = wb[g % 2]
        b = bb[g % 2]
        P.dma("pool", w[:], io.w_ada.ap(l * D * 6 * D + g * 512, [[6 * D, 128], [128 * 6 * D, 8], [1, 512]]))
        P.dma("sp", b[:], io.b_ada.ap(l * 6 * D + g * 512, [[0, 128], [1, 512]]))
        for v in range(2):
            p = ps[v]
            for k in range(8):
                lhsT = cvs.ap(k * 2 + v, [[0, 128]])
                P.mm(p[:], lhsT, w[:, k, :], start=(k == 0), stop=(k == 7))
            dst = mt[(g * 2 + v) % 4]
            if g in (2, 3, 8, 9):
                P.stt(dst[:], p[:], 1.0, b[:], ALU.add, ALU.add)
            else:
                P.tt(dst[:], p[:], b[:], ALU.add)
            P.dma("sp", sc.MOD[v:v + 1, g * 512:(g + 1) * 512], dst[0:1, :])
    P.release(m)


def phase_proj(P, io, sc, l, lbt, dbg=None):
    m = P.mark()
    modL = ModSlice(P, sc, 0, 0, 2048)
    modC = ModSlice(P, sc, 1, 0, 2048)
    W = Ctx()
    W.st = P.sbuf("st", [128, 64], F32)
    ident_f = P.sbuf("identf", [128, 128], F32)
    ident = P.sbuf("ident", [128, 128], BF16)
    P.dma("sp", ident_f[:], io.ident[:])
    P.copy(ident[:], ident_f[:])
    win = P.sbuf("win", [128, 8, PTOT], BF16)
    for k in range(8):
        P.dma("pool", win[:, k, :], io.w_in.ap(l * D * PTOT + k * 128 * PTOT, [[PTOT, 128], [1, PTOT]]))
    wuq_a = P.sbuf("wuqa", [128, 384], BF16)
    wuq_b = P.sbuf("wuqb", [64, 384], BF16)
    wukv = P.sbuf("wukv", [128, 512], BF16)
    P.dma("pool", wuq_a[:], io.w_uq.ap(l * 192 * 384, [[384, 128], [1, 384]]))
    P.dma("pool", wuq_b[:], io.w_uq.ap(l * 192 * 384 + 128 * 384, [[384, 64], [1, 384]]))
    P.dma("pool", wukv[:], io.w_ukv.ap(l * 128 * 512, [[512, 128], [1, 512]]))
    gn = P.sbuf("gains", [128, 640], F32)
    P.dma("sp", gn[:], io.gains.ap(l * 768, [[0, 128], [1, 640]]))

    W.pool = [(P.sbuf("sqp", [128, 256], F32), P.sbuf("stp", [128, 64], F32)) for _ in range(4)]
    Wb = Ctx()
    Wb.pool = [(P.sbuf("sqb", [128, 256], F32), P.sbuf("stb", [128, 64], F32)) for _ in range(2)]
    psNT = P.psum("psNT", [128, 4, 128], BF16)
    vva = [P.sbuf("vva", [128, 256], BF16) for _ in range(2)]
    xt = [P.sbuf("xt", [128, D], F32) for _ in range(2)]
    xsq = P.sbuf("xsq", [128, D], F32)
    sst = P.sbuf("sst", [128, 2], F32)
    xn = P.sbuf("xn", [128, D], BF16)
    xnT = P.sbuf("xnT", [128, 8, 128], BF16)
    psT = P.psum("psT", [128, 8, 128], BF16)
    psP = [P.psum("psP", [128, 512]) for _ in range(2)]
    pps = [P.sbuf("pp", [128, PTOT], F32) for _ in range(2)]
    ropes = [P.sbuf("rope", [128, 64], F32) for _ in range(2)]
    lat = P.sbuf("lat", [128, 352], BF16)
    latT = P.sbuf("latT", [128, 3, 128], BF16)
    psL = P.psum("psL", [128, 3, 128], BF16)
    psQ = P.psum("psQ", [128, 512])
    psKV = P.psum("psKV", [128, 512])
    qf = P.sbuf("qf", [128, 4, 96], F32)
    kf = P.sbuf("kf", [128, 4, 96], F32)
    krn = P.sbuf("krn", [128, 32], F32)
    tmpr = P.sbuf("tmpr", [128, 4, 32], F32)
    tmpk = P.sbuf("tmpk", [128, 32], F32)
    qb = P.sbuf("qb", [128, 4, 96], BF16)
    kb = P.sbuf("kb", [128, 4, 96], BF16)
    psQT = P.psum("psQT", [128, 8, 128], BF16)
    qkT = [P.sbuf("qkT", [96, 8, 128], BF16) for _ in range(2)]
    naqk = P.sbuf("naqk", [128, 512], BF16)
    naT = [P.sbuf("naT", [128, 4, 128], BF16) for _ in range(2)]
    vvs = [P.sbuf("vv", [128, 1024], BF16) for _ in range(2)]
    gls = [P.sbuf("gl", [128, 2304], F32) for _ in range(2)]
    tmpf = P.sbuf("tmpf", [128, 512], F32)
    W.fb = P.sbuf("fbuf", [128, 512], F32)

    groups = [(0, 352), (352, 512), (864, 512), (1376, 512), (1888, 512), (2400, 512), (2912, 512)]

    def stage1(i):
        isctx = i >= 32
        mod = modC if isctx else modL
        x = xt[i % 2]
        pp = pps[i % 2]
        if l == 0:
            src = io.ctx[(i - 32) * 128:(i - 31) * 128, :] if isctx else io.x[i * 128:(i + 1) * 128, :]
        else:
            src = sc.xres[i * 128:(i + 1) * 128, :]
        P.dma("sp", x[:], src)
        P.dma("sp", ropes[i % 2][:], io.rope[i * 128:(i + 1) * 128, :])
        ssx = sst[:, 0:1]
        P.act(xsq[:], x[:], AF.Square, accum=ssx)
        P.ts(ssx, ssx, 1.0 / D, EPS, ALU.mult, ALU.add)
        P.act(ssx, ssx, AF.Sqrt)
        P.recip(ssx, ssx)
        P.stt(xsq[:], x[:], ssx, mod[:, 1024:2048], ALU.mult, ALU.mult)
        P.tt(xn[:], xsq[:], mod[:, 0:1024], ALU.add)
        if l == 0:
            P.dma("sp", sc.xres[i * 128:(i + 1) * 128, :], x[:])
        for k in range(8):
            P.tr(psT[:, k, :], xn[:, k * 128:(k + 1) * 128], ident[:])
        P.copy(xnT[:, 0:4, :], psT[:, 0:4, :], eng="act")
        P.copy(xnT[:, 4:8, :], psT[:, 4:8, :], eng="dve")
        for gi, (c0, n) in enumerate(groups):
            ps = psP[gi % 2]
            for k in range(8):
                P.mm(ps[:, 0:n], xnT[:, k, :], win[:, k, c0:c0 + n], start=(k == 0), stop=(k == 7))
            P.copy(pp[:, c0:c0 + n], ps[:, 0:n], eng=("act" if gi % 2 == 0 else "dve"))
        if dbg is not None and "pp" in dbg:
            P.dma("sp", dbg["pp"][i * 128:(i + 1) * 128, :], pp[:])

    def stage2a(i):
        pp = pps[i % 2]
        rope = ropes[i % 2]
        vv = vvs[i % 2]
        gl = gls[i % 2]
        rms_groups(P, W, pp.ap(0, [[192, 1], [1, 192]]), 1, 192, gn.ap(0, [[0, 1], [1, 192]]),
                   lat.ap(0, [[192, 1], [1, 192]]))
        rms_groups(P, W, pp.ap(192, [[128, 1], [1, 128]]), 1, 128, gn.ap(192, [[0, 1], [1, 128]]),
                   lat.ap(192, [[128, 1], [1, 128]]))
        P.tr(psL[:, 0, :], lat[:, 0:128], ident[:])
        P.tr(psL[0:64, 1, :], lat[:, 128:192], ident[:])
        P.tr(psL[:, 2, :], lat[:, 192:320], ident[:])
        P.copy(latT[:, 0, :], psL[:, 0, :], eng="act")
        P.copy(latT[0:64, 1, :], psL[0:64, 1, :], eng="dve")
        P.copy(latT[:, 2, :], psL[:, 2, :], eng="act")
        P.mm(psQ[:, 0:384], latT[:, 0, :], wuq_a[:], start=True, stop=False)
        P.mm(psQ[:, 0:384], latT[0:64, 1, :], wuq_b[:], start=False, stop=True)
        P.mm(psKV[:], latT[:, 2, :], wukv[:], start=True, stop=True)
        rms_groups(P, W, psQ.ap(0, [[96, 4], [1, 64]]), 4, 64, gn.ap(320, [[0, 4], [1, 64]]),
                   qf.ap(0, [[96, 4], [1, 64]]), scale=MLA_SCALE)
        rms_groups(P, W, psQ.ap(64, [[96, 4], [1, 32]]), 4, 32, gn.ap(384, [[0, 4], [1, 32]]),
                   qf.ap(64, [[96, 4], [1, 32]]), scale=MLA_SCALE)
        rms_groups(P, W, psKV.ap(0, [[128, 4], [1, 64]]), 4, 64, gn.ap(416, [[0, 4], [1, 64]]),
                   kf.ap(0, [[96, 4], [1, 64]]))
        rms_groups(P, W, pp.ap(320, [[32, 1], [1, 32]]), 1, 32, gn.ap(480, [[0, 1], [1, 32]]),
                   krn.ap(0, [[32, 1], [1, 32]]))
        P.copy(vva[i % 2].ap(0, [[64, 4], [1, 64]]), psKV.ap(64, [[128, 4], [1, 64]]), eng="act")
        P.dma("sp", sc.VV[i * 128:(i + 1) * 128, 0:256], vva[i % 2][:])
        qr_ = lambda o: qf.ap(64 + o, [[96, 4], [16, 2], [1, 8]])
        tm_ = lambda o: tmpr.ap(o, [[32, 4], [16, 2], [1, 8]])
        P.copy(tm_(0), qr_(8))
        P.copy(tm_(8), qr_(0))
        Cb = rope.ap(0, [[0, 4], [1, 32]])
        Sb = rope.ap(32, [[0, 4], [1, 32]])
        P.tt(tmpr[:], tmpr[:], Sb, ALU.mult, eng="pool")
        P.tt(qf.ap(64, [[96, 4], [1, 32]]), qf.ap(64, [[96, 4], [1, 32]]), Cb, ALU.mult)
        P.tt(qf.ap(64, [[96, 4], [1, 32]]), qf.ap(64, [[96, 4], [1, 32]]), tmpr[:], ALU.add)
        P.copy(tmpk.ap(0, [[16, 2], [1, 8]]), krn.ap(8, [[16, 2], [1, 8]]))
        P.copy(tmpk.ap(8, [[16, 2], [1, 8]]), krn.ap(0, [[16, 2], [1, 8]]))
        P.tt(tmpk[:], tmpk[:], rope[:, 32:64], ALU.mult, eng="pool")
        P.tt(krn[:], krn[:], rope[:, 0:32], ALU.mult)
        P.tt(krn[:], krn[:], tmpk[:], ALU.add)
        P.copy(kf.ap(64, [[96, 4], [1, 32]]), krn.ap(0, [[0, 4], [1, 32]]))
        P.copy(qb[:], qf[:], eng="act")
        P.copy(kb[:], kf[:], eng="act")
        for h in range(4):
            P.tr(psQT[0:96, h, :], qb[:, h, :], ident[:])
            P.tr(psQT[0:96, 4 + h, :], kb[:, h, :], ident[:])
        qkTt = qkT[i % 2]
        P.copy(qkTt[:, 0:4, :], psQT[0:96, 0:4, :], eng="act")
        P.copy(qkTt[:, 4:8, :], psQT[0:96, 4:8, :], eng="dve")
        P.dma("sp", sc.mlaQT.ap(i * 128, [[T, 96], [96 * T, 4], [1, 128]]), qkTt[:, 0:4, :])
        P.dma("sp", sc.mlaKT.ap(i * 128, [[T, 96], [96 * T, 4], [1, 128]]), qkTt[:, 4:8, :])

    def stage2b(i):
        pp = pps[i % 2]
        rope = ropes[i % 2]
        vv = vvs[i % 2]
        gl = gls[i % 2]
        rms_groups(P, Wb, pp.ap(352, [[64, 4], [1, 64]]), 4, 64, gn.ap(512, [[0, 4], [1, 64]]),
                   naqk.ap(0, [[64, 4], [1, 64]]), scale=NA_SCALE)
        rms_groups(P, Wb, pp.ap(608, [[64, 4], [1, 64]]), 4, 64, gn.ap(576, [[0, 4], [1, 64]]),
                   naqk.ap(256, [[64, 4], [1, 64]]))
        for j in range(4):
            P.tr(psNT[:, j, :], naqk[:, j * 128:(j + 1) * 128], ident[:])
        naTt = naT[i % 2]
        P.copy(naTt[:, 0:2, :], psNT[:, 0:2, :], eng="act")
        P.copy(naTt[:, 2:4, :], psNT[:, 2:4, :], eng="dve")
        P.dma("sp", sc.naQT.ap(i * 128, [[T, 128], [128 * T, 2], [1, 128]]), naTt[:, 0:2, :])
        P.dma("sp", sc.naKT.ap(i * 128, [[T, 128], [128 * T, 2], [1, 128]]), naTt[:, 2:4, :])
        P.copy(vv[:, 256:512], pp[:, 864:1120], eng="act")
        P.copy(vv[:, 512:768], pp[:, 1632:1888], eng="dve")
        P.copy(vv[:, 768:1024], pp[:, 2912:3168], eng="act")
        P.dma("sp", sc.VV[i * 128:(i + 1) * 128, 256:1024], vv[:, 256:1024])

    def stage2c(i):
        pp = pps[i % 2]
        rope = ropes[i % 2]
        vv = vvs[i % 2]
        gl = gls[i % 2]
        P.copy(gl[:, 0:256], pp[:, 1120:1376], eng="dve")
        P.ts(gl[:, 256:512], pp[:, 1376:1632], 0.125, None, ALU.mult)
        P.act(gl[:, 512:768], pp[:, 2144:2400], AF.Silu)
        P.act(tmpf[:], pp[:, 2400:2912], AF.Sigmoid)
        hg_gates(P, W, lbt, l, tmpf, gl)
        P.act(gl[:, 1792:2048], pp[:, 1888:2144], AF.Silu)
        P.act(gl[:, 2048:2304], pp[:, 3168:3424], AF.Silu)
        P.dma("sp", sc.GL[i * 128:(i + 1) * 128, :], gl[:])


    stage1(0)
    for i in range(NT):
        sa = P.record(lambda: stage2a(i))
        sb_ = P.record(lambda: stage2b(i))
        sc_ = P.record(lambda: stage2c(i))
        s1 = P.record(lambda: stage1(i + 1)) if i + 1 < NT else []
        P.play(sa, sb_, sc_, s1)
    P.release(m)


def hg_gates(P, W, lbt, l, sig, gl):
    for d in range(2):
        s = sig[:, d * 256:(d + 1) * 256]
        f = W.fb[:, d * 256:(d + 1) * 256]
        P.tt(f, s, lbt[:, l, 256:512], ALU.mult)
        P.tt(f, f, lbt[:, l, 0:256], ALU.add)
        P.ts(gl[:, 768 + d * 256:1024 + d * 256], f, -1.0, 1.0, ALU.mult, ALU.add)
        P.ts(f, f, 1e-20, None, ALU.max)
        P.act(gl[:, 1280 + d * 256:1536 + d * 256], f, AF.Ln)


def phase_lb(P, io, lbt):
    m = P.mark()
    raw = P.sbuf("lbraw", [128, 4, 256], F32)
    e = P.sbuf("lbe", [128, 4, 256], F32)
    mx = P.sbuf("lbmx", [128, 256], F32)
    P.dma("sp", raw[:], io.hg_lb_raw.ap(0, [[0, 128], [256, 4], [1, 256]]))
    P.tt(mx[:], raw[:, 0, :], raw[:, 1, :], ALU.max)
    P.tt(mx[:], mx[:], raw[:, 2, :], ALU.max)
    P.tt(mx[:], mx[:], raw[:, 3, :], ALU.max)
    for l in range(4):
        P.tt(e[:, l, :], raw[:, l, :], mx[:], ALU.subtract)
    P.act(e[:], e[:], AF.Exp)
    P.tt(mx[:], e[:, 0, :], e[:, 1, :], ALU.add)
    P.tt(mx[:], mx[:], e[:, 2, :], ALU.add)
    P.tt(mx[:], mx[:], e[:, 3, :], ALU.add)
    P.recip(mx[:], mx[:])
    for l in range(4):
        P.tt(e[:, l, :], e[:, l, :], mx[:], ALU.mult)
    P.memset(lbt[:, 0, 0:256], 0.0)
    P.copy(lbt[:, 1, 0:256], e[:, 1, :])
    P.tt(lbt[:, 2, 0:256], lbt[:, 1, 0:256], e[:, 2, :], ALU.add)
    P.tt(lbt[:, 3, 0:256], lbt[:, 2, 0:256], e[:, 3, :], ALU.add)
    for l in range(4):
        P.ts(lbt[:, l, 256:512], lbt[:, l, 0:256], -1.0, 1.0, ALU.mult, ALU.add)
    P.release(m)


def attn_epilogue(P, A, OT, n, yrow0, q0, sc):
    osb = A.osb[A.ei % 2]
    yb = A.yb[A.ei % 2]
    A.ei += 1
    P.copy(osb[:, 0:n], OT[:, 0:n], eng="dve")
    P.mm(A.psB[:, 0:n], A.ones[64:65, 0:64], osb[64:65, 0:n], start=True, stop=True)
    P.recip(A.rb[:, 0:n], A.psB[:, 0:n])
    P.tt(yb[:, 0:n], osb[0:64, 0:n], A.rb[:, 0:n], ALU.mult)
    P.dma("sp", sc.YT[yrow0:yrow0 + 64, q0:q0 + n], yb[:, 0:n])


def attn_common(P):
    A = Ctx()
    A.ei = 0
    A.osb = [P.sbuf("osb", [65, 512], F32) for _ in range(2)]
    A.yb = [P.sbuf("yb", [64, 512], BF16) for _ in range(2)]
    A.rb = P.sbuf("rb", [64, 512], F32)
    A.ones = P.sbuf("ones", [65, 64], F32)
    P.memset(A.ones[:], 1.0)
    A.psB = P.psum("psB", [64, 512])
    A.vt = P.sbuf("vt", [128, NT, 4, 65], BF16)
    return A


def load_v(P, A, sc, col0):
    P.memset(A.vt.ap(64, [[65, NT * 4], [1, 1]]), 1.0)
    for i in range(NT):
        P.dma("sp", A.vt[:, i, :, 0:64], sc.VV.ap(i * 128 * 1024 + col0, [[1024, 128], [64, 4], [1, 64]]))


def phase_mla(P, io, sc):
    m = P.mark()
    A = attn_common(P)
    load_v(P, A, sc, 0)
    KT = P.sbuf("KT", [96, 4, T], BF16)
    QT = P.sbuf("QT", [96, 4, T], BF16)
    for h in range(4):
        P.dma("sp", KT[:, h, :], sc.mlaKT[h, :, :])
        P.dma("sp", QT[:, h, :], sc.mlaQT[h, :, :])
    NB = 5
    LAG = 3
    psS = [P.psum("psS", [128, 512]) for _ in range(NB)]
    psO = [P.psum("psO", [65, 512]) for _ in range(2)]
    PT = [P.sbuf("PT", [128, 512], BF16) for _ in range(NB)]
    it = 0
    ci = 0
    for h in range(4):
        for qc in range(9):
            q0 = qc * 512
            n = 512 if qc < 8 else 256
            kts = list(range(NT)) if qc < 8 else [32, 33]
            OT = psO[ci % 2]
            ci += 1
            nk = len(kts)
            slots = []
            for step in range(nk + LAG):
                if step < nk:
                    kt = kts[step]
                    S = psS[it % NB]
                    pt = PT[it % NB]
                    it += 1
                    slots.append(pt)
                    P.mm(S[:, 0:n], KT[:, h, kt * 128:(kt + 1) * 128], QT[:, h, q0:q0 + n])
                    P.act(pt[:, 0:n], S[:, 0:n], AF.Exp)
                j = step - LAG
                if j >= 0:
                    P.mm(OT[:, 0:n], A.vt[:, kts[j], h, :], slots[j][:, 0:n], start=(j == 0), stop=(j == nk - 1))
            attn_epilogue(P, A, OT, n, h * 64, q0, sc)
    P.release(m)


def na_bias_index():
    dr = np.zeros((5, 5, 128, 128), np.int64)
    dc = np.zeros((5, 5, 128, 128), np.int64)
    va = np.zeros((5, 5, 128, 128), bool)
    qq = np.arange(128)
    kk = np.arange(128)
    for pat, i in enumerate([2, 0, 1, 30, 31]):
        kb = min(max(i - 2, 0), 27)
        for j in range(5):
            r = 2 * i + qq[:, None] // 64
            wq = qq[:, None] % 64
            krow = 2 * (kb + j) + kk[None, :] // 64
            wk = kk[None, :] % 64
            rs = np.clip(r - 4, 0, 56)
            cs = np.clip(wq - 8, 0, 48)
            v = (krow >= rs) & (krow < rs + 8) & (wk >= cs) & (wk < cs + 16)
            va[pat, j] = v
            dr[pat, j] = np.clip(krow - r + 7, 0, 14)
            dc[pat, j] = np.clip(wk - wq, -15, 15) + 15
    return dr, dc, va


def na_bias_tables(rpb):
    dr, dc, va = na_bias_index()
    L = rpb.shape[0]
    out = np.empty((L, 5, 4, 5, 128, 128), np.float32)
    for l in range(L):
        for h in range(4):
            out[l, :, h] = np.where(va, rpb[l, h][dr, dc], np.float32(NEG))
    return np.ascontiguousarray(out.transpose(0, 4, 1, 2, 3, 5).reshape(L, 128, 100, 128))


def phase_na(P, io, sc, l):
    m = P.mark()
    A = attn_common(P)
    load_v(P, A, sc, 256)
    KT = P.sbuf("nKT", [128, 2, T], BF16)
    QT = P.sbuf("nQT", [128, 2, T], BF16)
    for hp in range(2):
        P.dma("sp", KT[:, hp, :], sc.naKT[hp * 128:(hp + 1) * 128, :])
        P.dma("sp", QT[:, hp, :], sc.naQT[hp * 128:(hp + 1) * 128, :])
    BT = P.sbuf("BT", [128, 100, 128], BF16)
    for c in range(4):
        P.dma("pool", BT[:, c * 25:(c + 1) * 25, :], io.na_bias.ap(l * 128 * 12800 + c * 25 * 128, [[12800, 128], [128, 25], [1, 128]]))
    identf = P.sbuf("identf", [128, 128], F32)
    ident = P.sbuf("ident", [128, 128], BF16)
    P.dma("sp", identf[:], io.ident[:])
    P.copy(ident[:], identf[:])
    psS = [P.psum("psS", [128, 8, 128]) for _ in range(2)]
    psO = [P.psum("psO", [65, 512]) for _ in range(2)]
    PT = [P.sbuf("PT", [128, 8, 128], BF16) for _ in range(2)]
    it = 0
    pend = []

    def flush():
        while pend:
            pend.pop(0)()

    for h in range(4):
        pb = (h % 2) * 64
        hp = h // 2
        for grp in range(9):
            OT = psO[(h * 9 + grp) % 2]
            nsub = 4 if grp < 8 else 2
            for s in range(nsub):
                i = grp * 4 + s
                q0 = i * 128
                S = psS[it % 2]
                pt = PT[it % 2]
                it += 1
                if grp < 8:
                    kb = min(max(i - 2, 0), 27)
                    pat = {0: 1, 1: 2, 30: 3, 31: 4}.get(i, 0)
                    kts = [kb + j for j in range(5)] + [32, 33]
                    for j in range(5):
                        kt = kb + j
                        P.mm(S[:, j, :], KT[pb:pb + 64, hp, kt * 128:(kt + 1) * 128], QT[pb:pb + 64, hp, q0:q0 + 128],
                             start=True, stop=False)
                        P.mm(S[:, j, :], BT[:, (pat * 4 + h) * 5 + j, :], ident[:], start=False, stop=True)
                    for j in (5, 6):
                        kt = 32 + j - 5
                        P.mm(S[:, j, :], KT[pb:pb + 64, hp, kt * 128:(kt + 1) * 128], QT[pb:pb + 64, hp, q0:q0 + 128])
                else:
                    kts = [32, 33]
                    for j in range(2):
                        kt = 32 + j
                        P.mm(S[:, j, :], KT[pb:pb + 64, hp, kt * 128:(kt + 1) * 128], QT[pb:pb + 64, hp, q0:q0 + 128])
                nk = len(kts)
                P.act(pt[:, 0:nk, :], S[:, 0:nk, :], AF.Exp)
                flush()

                def pv(OT=OT, s=s, kts=kts, pt=pt, h=h, nk=nk, last=(s == nsub - 1), nsub=nsub, grp=grp):
                    for j, kt in enumerate(kts):
                        P.mm(OT[:, s * 128:(s + 1) * 128], A.vt[:, kt, h, :], pt[:, j, :], start=(j == 0), stop=(j == nk - 1))
                    if last:
                        attn_epilogue(P, A, OT, nsub * 128, 256 + h * 64, grp * 512, sc)
                pend.append(pv)
    flush()
    P.release(m)


NCH = (2, 4)


def gla_consts():
    s = np.arange(128)[:, None]
    t = np.arange(128)[None, :]
    c = {}
    masks = np.zeros((2, 2, 128, 128), np.float32)
    ci = np.zeros((128, 2, 8), np.float32)
    for mx in range(2):
        cs_ = 128 // NCH[mx]
        same = (s // cs_) == (t // cs_)
        masks[mx, 0] = same & (s <= t)
        masks[mx, 1] = same & (s >= t)
        for cc in range(NCH[mx]):
            ci[cc * cs_:(cc + 1) * cs_, mx, cc] = 1
    c["gla_mask"] = np.ascontiguousarray(masks.transpose(2, 0, 1, 3).reshape(128, 4 * 128))
    c["gla_ci"] = np.ascontiguousarray(ci.reshape(128, 16))
    cs_ = 128 // NCH[1]
    same = (s // cs_) == (t // cs_)
    mm = np.zeros((2, 3, 128, 128), np.float32)
    for d in range(2):
        le = (s <= t) if d == 0 else (s >= t)
        mid = (t // cs_) * cs_ + (cs_ // 2 - 1 if d == 0 else cs_ // 2)
        lemid = (s <= mid) if d == 0 else (s >= mid)
        MA = (same & le).astype(np.float32)
        Mm = (same & lemid).astype(np.float32)
        M3 = (same & ~le).astype(np.float32)
        mm[d, 0] = MA - Mm
        mm[d, 1] = MA
        mm[d, 2] = M3
    c["gla_mm"] = np.ascontiguousarray(mm.transpose(2, 0, 1, 3).reshape(128, 6 * 128))
    j = np.arange(8, dtype=np.float64)
    lg = np.log1p(-np.exp2(-5.0 - j))
    lgd = [lg[0::2], lg[1::2]]
    tab = np.zeros((2, 4, 128, 256), np.float64)
    dec = np.zeros((64, 2, 4, 2), np.float64)
    tt = np.arange(128)
    for d in range(2):
        p = (tt % 64) if d == 0 else 63 - (tt % 64)
        for h in range(4):
            g = lgd[d][h]
            A = (p + 1) * g
            Amid = 32 * g
            Alast = 64 * g
            cols = slice(h * 64, (h + 1) * 64)
            tab[d, 0, :, cols] = np.exp(A - Amid)[:, None]
            tab[d, 1, :, cols] = np.exp(Amid - A)[:, None]
            tab[d, 2, :, cols] = np.exp(A)[:, None]
            tab[d, 3, :, cols] = np.exp(Alast - A)[:, None]
            dec[:, d, h, :] = np.exp(Alast)
    c["ret_tab"] = np.ascontiguousarray(tab.transpose(2, 0, 1, 3).reshape(128, 8 * 256)).astype(np.float32)
    c["ret_dec"] = np.ascontiguousarray(dec.reshape(64, 16)).astype(np.float32)
    return c


def phase_gla(P, io, sc, l):
    m = P.mark()
    W = Ctx()
    W.sq = P.sbuf("sq", [128, 1024], F32)
    W.st = P.sbuf("st", [128, 64], F32)
    identf = P.sbuf("identf", [128, 128], F32)
    ident = P.sbuf("ident", [128, 128], BF16)
    P.dma("sp", identf[:], io.ident[:])
    P.copy(ident[:], identf[:])
    mask = P.sbuf("gmask", [128, 4, 128], F32)
    P.dma("sp", mask[:], io.gla_mask[:])
    gmm = P.sbuf("gmm", [128, 6, 128], F32)
    P.dma("sp", gmm[:], io.gla_mm[:])
    gci = P.sbuf("gci", [128, 2, 8], F32)
    P.dma("sp", gci[:], io.gla_ci[:])
    rtab = P.sbuf("rtab", [128, 8, 256], F32)
    P.dma("sp", rtab[:], io.ret_tab[:])
    rdec = P.sbuf("rdec", [64, 2, 4, 2], F32)
    P.dma("sp", rdec[:], io.ret_dec[:])
    gout = P.sbuf("gout", [128, 128], F32)
    P.dma("sp", gout[:], io.gains.ap(l * 768 + 640, [[0, 128], [1, 128]]))

    glt = [P.sbuf("glt", [128, 2304], F32) for _ in range(2)]
    vts = [P.sbuf("gvt", [128, 512], BF16) for _ in range(2)]
    oft = [P.sbuf("oft", [128, 512], F32) for _ in range(2)]
    E = P.sbuf("E", [128, 4, 256], F32)
    r1c = P.sbuf("r1c", [128, 256], F32)
    hdec = [P.sbuf("hdec", [64, 4, 8], F32) for _ in range(2)]
    qk = P.sbuf("qk", [128, 3, 256], BF16)
    khf = P.sbuf("khf", [128, 256], F32)
    khm = [[P.sbuf("khm", [128, NCH[mx], 256], BF16) for _ in range(2)] for mx in range(2)]
    TT = P.sbuf("TT", [64, 2, 4, 128], BF16)
    QhTm = [[P.sbuf("QhTm", [64, 4, NCH[mx], 128], BF16) for _ in range(2)] for mx in range(2)]
    for mx in range(2):
        for p_ in range(2):
            P.memset(QhTm[mx][p_][:], 0.0)
    Sm = [[P.sbuf("Sm", [128, 4, 128], BF16) for _ in range(2)] for mx in range(2)]
    S = [P.sbuf("S", [64, 4, 64], F32) for _ in range(2)]
    Sbf = [P.sbuf("Sbf", [64, 4, 64], BF16) for _ in range(8)]
    osum = P.sbuf("osum", [128, 256], F32)
    yt = [P.sbuf("yt", [128, 512], BF16) for _ in range(2)]
    psR = P.psum("psR", [128, 4, 256])
    psD = P.psum("psD", [64, 4, 8])
    psTQ = P.psum("psTQ", [64, 2, 4, 128], BF16)
    psTH = P.psum("psTH", [64, 4, 128], BF16)
    psDS = P.psum("psDS", [64, 8, 64])
    psSc = P.psum("psSc", [128, 4, 128])
    psO = P.psum("psO", [128, 256])

    def front(d, n_, i, mx):
        p_ = n_ % 2
        gl, vt, of = glt[p_], vts[p_], oft[p_]
        NC = NCH[mx]
        CS = 128 // NC
        if mx == 0:
            P.dma("sp", gl[:], sc.GL[i * 128:(i + 1) * 128, :])
            P.dma("sp", vt[:], sc.VV[i * 128:(i + 1) * 128, 512:1024])
            if d == 1:
                P.dma("sp", of[:], sc.OF[i * 128:(i + 1) * 128, :])
            q = gl[:, 0:256]
            k = gl[:, 256:512]
            Ev = lambda a: rtab[:, d * 4 + a, :]
        else:
            q = gl[:, 512:768]
            k = gl[:, 768 + d * 256:1024 + d * 256]
            lf0 = 1280 + d * 256
            for a in range(3):
                P.mm(psR[:, a, :], gmm[:, d * 3 + a, :], gl[:, lf0:lf0 + 256])
            P.ts(r1c[:], psR[:, 0, :], 43.0, -43.0, ALU.min, ALU.max)
            P.act(E[:, 0, :], r1c[:], AF.Exp)
            P.act(E[:, 1, :], r1c[:], AF.Exp, scale=-1.0)
            P.act(E[:, 2, :], psR[:, 1, :], AF.Exp)
            P.act(E[:, 3, :], psR[:, 2, :], AF.Exp)
            for h in range(4):
                P.mm(psD[:, h, :], gl[:, lf0 + h * 64:lf0 + (h + 1) * 64], gci[:, 1, :])
            P.act(hdec[p_][:], psD[:], AF.Exp)
            Ev = lambda a: E[:, a, :]
        P.tt(qk[:, 0, :], q, Ev(0), ALU.mult)
        P.tt(qk[:, 1, :], k, Ev(1), ALU.mult)
        P.tt(qk[:, 2, :], q, Ev(2), ALU.mult, eng="pool")
        P.tt(khf[:], k, Ev(3), ALU.mult, eng="pool")
        P.tt(khm[mx][p_][:], khf.ap(0, [[0, NC], [1, 256]]), gci.ap(mx * 8, [[1, NC], [0, 256]]), ALU.mult, eng="pool")
        for h in range(4):
            P.tr(psTQ[:, 0, h, :], qk[:, 0, h * 64:(h + 1) * 64], ident[:])
            P.tr(psTQ[:, 1, h, :], qk[:, 1, h * 64:(h + 1) * 64], ident[:])
            P.tr(psTH[:, h, :], qk[:, 2, h * 64:(h + 1) * 64], ident[:])
        P.copy(TT[:], psTQ[:], eng="act")
        P.copy(QhTm[mx][p_].ap(0, [[NC * 128, 4], [128 + CS, NC], [1, CS]]), psTH.ap(0, [[128, 4], [CS, NC], [1, CS]]), eng="dve")
        for h in range(4):
            P.mm(psSc[:, h, :], TT[:, 1, h, :], TT[:, 0, h, :])
        P.tt(Sm[mx][p_][:], psSc[:], mask.ap((mx * 2 + d) * 128, [[0, 4], [1, 128]]), ALU.mult)

    def back(d, n_, i, mx):
        p_ = n_ % 2
        gl, vt, of = glt[p_], vts[p_], oft[p_]
        NC = NCH[mx]
        corder = list(range(NC)) if d == 0 else list(range(NC - 1, -1, -1))
        if mx == 0:
            dec = lambda c: rdec.ap(d * 8 + c, [[2, 4], [0, 64]])
        else:
            dec = lambda c: hdec[p_].ap(c, [[8, 4], [0, 64]])
        vh = lambda h: vt[:, mx * 256 + h * 64:mx * 256 + (h + 1) * 64]
        St = S[mx]
        for n2, c in enumerate(corder):
            if n2 % 2 == 0:
                for c2 in corder[n2:n2 + 2]:
                    for h in range(4):
                        P.mm(psDS[:, (c2 % 2) * 4 + h, :], khm[mx][p_][:, c2, h * 64:(h + 1) * 64], vh(h))
            P.copy(Sbf[n2][:], St[:], eng="act")
            P.tt(St[:], St[:], dec(c), ALU.mult)
            P.tt(St[:], St[:], psDS[:, (c % 2) * 4:(c % 2) * 4 + 4, :], ALU.add)
        for h in range(4):
            oc = slice(h * 64, (h + 1) * 64)
            P.mm(psO[:, oc], Sm[mx][p_][:, h, :], vh(h), start=True, stop=False)
            for n2, c in enumerate(corder):
                P.mm(psO[:, oc], QhTm[mx][p_][:, h, c, :], Sbf[n2][:, h, :], start=False, stop=(n2 == NC - 1))
        if d == 0:
            P.copy(of[:, mx * 256:(mx + 1) * 256], psO[:], eng="act")
            if mx == 1:
                P.dma("sp", sc.OF[i * 128:(i + 1) * 128, :], of[:])
        else:
            ytt = yt[p_]
            P.tt(osum[:], psO[:], of[:, mx * 256:(mx + 1) * 256], ALU.add)
            rms_groups(P, W, osum.ap(0, [[64, 4], [1, 64]]), 4, 64, gout.ap(mx * 64, [[0, 4], [1, 64]]),
                       osum.ap(0, [[64, 4], [1, 64]]))
            P.tt(ytt[:, mx * 256:(mx + 1) * 256], osum[:], gl[:, 1792 + mx * 256:2048 + mx * 256], ALU.mult)
            if mx == 1:
                P.dma("sp", sc.Y[i * 128:(i + 1) * 128, :], ytt[:])

    for d in range(2):
        order = [32, 33] + list(range(32)) if d == 0 else [33, 32] + list(range(31, -1, -1))
        for mx in range(2):
            P.memset(S[mx][:], 0.0)
        units = [(d, n_, i, mx) for n_, i in enumerate(order) for mx in range(2)]
        front(*units[0])
        for u, un in enumerate(units):
            sb = P.record(lambda: back(*un))
            sf = P.record(lambda: front(*units[u + 1])) if u + 1 < len(units) else []
            P.play(sb, sf)
    P.release(m)


NSLOT = NEXP * CAP


def moe_consts():
    c = {}
    s = np.arange(128)[:, None]
    t = np.arange(128)[None, :]
    c["moe_U"] = (s < t).astype(np.float32)
    c["moe_eoff"] = np.broadcast_to((np.arange(NEXP) * CAP).astype(np.float32), (128, NEXP)).copy()
    return c


def phase_out(P, io, sc, l, RT):
    m = P.mark()
    modL = ModSlice(P, sc, 0, 2048, 5120)
    modC = ModSlice(P, sc, 1, 2048, 5120)
    W = Ctx()
    W.sq = P.sbuf("sq", [128, 1024], F32)
    W.st = P.sbuf("st", [128, 64], F32)
    identf = P.sbuf("identf", [128, 128], F32)
    ident = P.sbuf("ident", [128, 128], BF16)
    P.dma("sp", identf[:], io.ident[:])
    P.copy(ident[:], identf[:])
    wout = P.sbuf("wout", [128, 8, D], BF16)
    for k in range(8):
        P.dma("pool", wout[:, k, :], io.w_out.ap(l * D * D + k * 128 * D, [[D, 128], [1, D]]))
    wr = P.sbuf("wr", [128, 8, 36], F32)
    P.dma("sp", wr[:], io.moe_wr.ap(l * D * 36, [[36, 128], [128 * 36, 8], [1, 36]]))
    br = P.sbuf("br", [128, 36], F32)
    P.dma("sp", br[:], io.moe_br.ap(l * 36, [[0, 128], [1, 36]]))
    Uf = P.sbuf("Uf", [128, 128], F32)
    U = P.sbuf("U", [128, 128], BF16)
    P.dma("sp", Uf[:], io.moe_U[:])
    P.copy(U[:], Uf[:])
    onesb = P.sbuf("onesb", [128, 128], BF16)
    P.memset(onesb[:], 1.0)
    eoff = P.sbuf("eoff", [128, NEXP], F32)
    P.dma("sp", eoff[:], io.moe_eoff[:])
    cnt = P.sbuf("cnt", [128, NEXP], F32)
    P.memset(cnt[:], 0.0)

    yts = [P.sbuf("ytok", [128, 512], BF16) for _ in range(2)]
    yT = [P.sbuf("yT", [128, 8, 128], BF16) for _ in range(2)]
    xt = [P.sbuf("xo", [128, D], F32) for _ in range(2)]
    hfs = [P.sbuf("hf", [128, D], F32) for _ in range(2)]
    Wr = Ctx()
    Wr.st = P.sbuf("str", [128, 64], F32)
    hb = [P.sbuf("hb", [128, D], BF16) for _ in range(2)]
    hT = P.sbuf("hT", [128, 8, 128], F32)
    lg = P.sbuf("lg", [128, 36], F32)
    r = P.sbuf("rw", [128, 12, 32], F32)
    selb = P.sbuf("selb", [128, NEXP], BF16)
    psT = P.psum("psT", [128, 4, 128], BF16)
    psA = [P.psum("psA", [128, 512]) for _ in range(2)]
    psH = [P.psum("psH", [128, 4, 128]) for _ in range(2)]
    psL = P.psum("psL", [128, 36])
    psP = P.psum("psPos", [128, NEXP])

    def stageA(i):
        hf = hfs[i % 2]
        isctx = i >= 32
        mod = modC if isctx else modL
        yt, yTt, x, hbt = yts[i % 2], yT[i % 2], xt[i % 2], hb[i % 2]
        P.dma("sp", yt[:], sc.Y[i * 128:(i + 1) * 128, :])
        P.dma("sp", yTt[:, 0:4, :], sc.YT.ap(i * 128, [[T, 128], [128 * T, 4], [1, 128]]))
        P.dma("sp", x[:], sc.xres[i * 128:(i + 1) * 128, :])
        for c in range(4):
            P.tr(psT[:, c, :], yt[:, c * 128:(c + 1) * 128], ident[:])
        P.copy(yTt[:, 4:8, :], psT[:], eng="act")
        for half in range(2):
            ps = psA[half]
            for c in range(8):
                P.mm(ps[:], yTt[:, c, :], wout[:, c, half * 512:(half + 1) * 512], start=(c == 0), stop=(c == 7))
            hs = slice(half * 512, (half + 1) * 512)
            P.tt(W.sq[:, hs], ps[:], mod[:, 2048 + half * 512:2048 + (half + 1) * 512], ALU.mult)
            P.tt(x[:, hs], x[:, hs], W.sq[:, hs], ALU.add)
        P.dma("sp", sc.xres[i * 128:(i + 1) * 128, :], x[:])
        ssx = W.st[:, 48:49]
        P.act(W.sq[:], x[:], AF.Square, accum=ssx)
        P.ts(ssx, ssx, 1.0 / D, EPS, ALU.mult, ALU.add)
        P.act(ssx, ssx, AF.Sqrt)
        P.recip(ssx, ssx)
        P.stt(W.sq[:], x[:], ssx, mod[:, 4096:5120], ALU.mult, ALU.mult)
        P.tt(hf[:], W.sq[:], mod[:, 3072:4096], ALU.add)
        P.copy(hbt[:], hf[:], eng="act")

    def stageB(i):
        hf = hfs[i % 2]
        hbt = hb[i % 2]
        for rnd in range(2):
            ph = psH[rnd]
            for k in range(4):
                P.tr(ph[:, k, :], hf[:, (rnd * 4 + k) * 128:(rnd * 4 + k + 1) * 128], identf[:])
            P.copy(hT[:, rnd * 4:(rnd + 1) * 4, :], ph[:], eng=("act" if rnd == 0 else "dve"))
        for k in range(8):
            P.mm(psL[:], hT[:, k, :], wr[:, k, :], start=(k == 0), stop=(k == 7))
        P.tt(lg[:], psL[:], br[:], ALU.add)
        route_tile(P, Wr, i, lg, r, selb, U, onesb, eoff, cnt, psP, RT)
        for a in range(2):
            idx = RT.slot[:, i, a:a + 1]
            P.dma("pool", sc.XS[:, :], hbt[:], extra_reads=[idx],
                  indirect=lambda e, idx=idx, hbt=hbt: e.indirect_dma_start(
                      out=sc.XS.t[:, :], out_offset=bass.IndirectOffsetOnAxis(ap=idx.ap, axis=0),
                      in_=hbt.t[:, :], in_offset=None, bounds_check=P.breg, oob_is_err=False))

    stageA(0)
    for i in range(NT):
        sB = P.record(lambda: stageB(i))
        sA = P.record(lambda: stageA(i + 1)) if i + 1 < NT else []
        P.play(sB, sA)
    P.release(m)


def route_tile(P, W, i, lg, r, selb, U, onesb, eoff, cnt, psP, RT):
    BIG = 1.0e4
    g4 = lg[:, 0:4]
    f32v = lg[:, 4:36]
    gmax = W.st[:, 0:1]
    ngmax = W.st[:, 1:2]
    sg = W.st[:, 2:3]
    m1 = W.st[:, 3:4]
    m2 = W.st[:, 4:5]
    nm1 = W.st[:, 5:6]
    se = W.st[:, 6:7]
    P.red(gmax, g4, op=ALU.max)
    P.ts(ngmax, gmax, -1.0, None, ALU.mult)
    oh = r[:, 0, 0:4]
    P.ts(oh, g4, gmax, None, ALU.is_equal)
    eg = r[:, 0, 8:12]
    P.act(eg, g4, AF.Exp, bias=ngmax, accum=sg)
    pen = r[:, 0, 16:20]
    P.ts(pen, oh, BIG, -BIG, ALU.mult, ALU.add)
    lfm = r[:, 1, :]
    P.tt(r.ap(32, [[8, 4], [1, 8]]), lg.ap(4, [[8, 4], [1, 8]]), r.ap(16, [[1, 4], [0, 8]]), ALU.add)
    P.red(m1, lfm, op=ALU.max)
    eq1 = r[:, 2, :]
    P.ts(eq1, lfm, m1, None, ALU.is_equal)
    lfm2 = r[:, 3, :]
    P.stt(lfm2, eq1, -BIG, lfm, ALU.mult, ALU.add)
    P.red(m2, lfm2, op=ALU.max)
    sel = r[:, 4, :]
    P.ts(sel, lfm, m2, None, ALU.is_ge)
    eq2 = r[:, 5, :]
    P.tt(eq2, sel, eq1, ALU.subtract)
    P.ts(nm1, m1, -1.0, None, ALU.mult)
    ee = r[:, 6, :]
    P.act(ee, lfm, AF.Exp, bias=nm1)
    P.tt(ee, ee, sel, ALU.mult)
    P.red(se, ee)
    P.tt(se, se, sg, ALU.mult)
    P.recip(se, se)
    P.ts(ee, ee, se, None, ALU.mult)
    P.copy(selb[:], sel)
    P.mm(psP[:], U[:], selb[:], start=True, stop=True)
    pos = r[:, 7, :]
    P.tt(pos, psP[:], cnt[:], ALU.add)
    P.mm(psP[:], onesb[:], selb[:], start=True, stop=True)
    P.tt(cnt[:], cnt[:], psP[:], ALU.add)
    ok = r[:, 8, :]
    P.ts(ok, pos, float(CAP), None, ALU.is_lt)
    sv = r[:, 9, :]
    P.tt(sv, pos, eoff[:], ALU.add)
    P.tt(sv, sv, ok, ALU.mult)
    P.ts(ok, ok, -float(NSLOT + 8), float(NSLOT + 8), ALU.mult, ALU.add)
    P.tt(sv, sv, ok, ALU.add)
    tmp = r[:, 10, :]
    sf = W.st[:, 8:10]
    for a, eq in enumerate((eq1, eq2)):
        P.tt(tmp, sv, eq, ALU.mult)
        P.red(W.st[:, 8 + a:9 + a], tmp)
        P.tt(tmp, ee, eq, ALU.mult)
        P.red(RT.gate[:, i, a:a + 1], tmp)
    P.ts(W.st[:, 10:12], sf, float(NSLOT), None, ALU.is_lt)
    P.tt(RT.gate[:, i, :], RT.gate[:, i, :], W.st[:, 10:12], ALU.mult)
    P.copy(RT.slot[:, i, :], sf)


def phase_experts(P, io, sc, l):
    m = P.mark()
    identf = P.sbuf("identf", [128, 128], F32)
    ident = P.sbuf("ident", [128, 128], BF16)
    P.dma("sp", identf[:], io.ident[:])
    P.copy(ident[:], identf[:])
    w1 = [P.sbuf("w1", [128, 8, FF], BF16) for _ in range(2)]
    w3 = [P.sbuf("w3", [128, 8, FF], BF16) for _ in range(2)]
    w2 = [P.sbuf("w2", [128, 4, D], BF16) for _ in range(2)]
    xs = [P.sbuf("xs", [128, D], BF16) for _ in range(3)]
    xT = [P.sbuf("xT", [128, 8, 512], BF16) for _ in range(2)]
    hT = [P.sbuf("hTe", [128, 4, 512], BF16) for _ in range(2)]
    s1 = [P.sbuf("s1", [128, 512], F32) for _ in range(2)]
    ys = [P.sbuf("ys", [128, D], BF16) for _ in range(2)]
    psT = [P.psum("psT", [128, 8, 128], BF16) for _ in range(2)]
    ps1 = [P.psum("ps1", [128, 512]) for _ in range(2)]
    ps3 = [P.psum("ps3", [128, 512]) for _ in range(2)]
    psY = [P.psum("psY", [128, 512]) for _ in range(2)]
    cnt = {"nx": 0, "ny": 0}
    groups = [(e, g) for e in range(NEXP) for g in range(CAP // 512)]

    def load_w(e):
        a, b3, c2 = w1[e % 2], w3[e % 2], w2[e % 2]
        base = (l * NEXP + e)
        for k in range(0, 8, 2):
            P.dma("pool", a[:, k:k + 2, :], io.moe_w1.ap(base * D * FF + k * 128 * FF, [[FF, 128], [128 * FF, 2], [1, FF]]))
            P.dma("pool", b3[:, k:k + 2, :], io.moe_w3.ap(base * D * FF + k * 128 * FF, [[FF, 128], [128 * FF, 2], [1, FF]]))
        for k in range(4):
            P.dma("pool", c2[:, k, :], io.moe_w2.ap(base * FF * D + k * 128 * D, [[D, 128], [1, D]]))

    def prep(gi):
        e, g = groups[gi]
        if g == 0:
            load_w(e)
        xTt = xT[gi % 2]
        for t4 in range(4):
            row0 = e * CAP + g * 512 + t4 * 128
            xst = xs[cnt["nx"] % 3]
            pT = psT[cnt["nx"] % 2]
            cnt["nx"] += 1
            P.dma("sp", xst[:], sc.XS[row0:row0 + 128, :])
            for k in range(8):
                P.tr(pT[:, k, :], xst[:, k * 128:(k + 1) * 128], ident[:])
            P.copy(xTt[:, 0:4, t4 * 128:(t4 + 1) * 128], pT[:, 0:4, :], eng="act")
            P.copy(xTt[:, 4:8, t4 * 128:(t4 + 1) * 128], pT[:, 4:8, :], eng="dve")

    def compute(gi):
        e, g = groups[gi]
        a, b3, c2 = w1[e % 2], w3[e % 2], w2[e % 2]
        xTt = xT[gi % 2]
        hTt = hT[gi % 2]
        for fc in range(4):
            p1, p3, s = ps1[fc % 2], ps3[fc % 2], s1[fc % 2]
            for k in range(8):
                P.mm(p1[:], a[:, k, fc * 128:(fc + 1) * 128], xTt[:, k, :], start=(k == 0), stop=(k == 7))
            for k in range(8):
                P.mm(p3[:], b3[:, k, fc * 128:(fc + 1) * 128], xTt[:, k, :], start=(k == 0), stop=(k == 7))
            P.act(s[:], p1[:], AF.Silu)
            P.tt(hTt[:, fc, :], s[:], p3[:], ALU.mult)
        for t4 in range(4):
            row0 = e * CAP + g * 512 + t4 * 128
            yst = ys[cnt["ny"] % 2]
            cnt["ny"] += 1
            for half in range(2):
                py = psY[half]
                for fc in range(4):
                    P.mm(py[:], hTt[:, fc, t4 * 128:(t4 + 1) * 128], c2[:, fc, half * 512:(half + 1) * 512],
                         start=(fc == 0), stop=(fc == 3))
                P.copy(yst[:, half * 512:(half + 1) * 512], py[:], eng=("act" if half == 0 else "dve"))
            P.dma("act", sc.YS[row0:row0 + 128, :], yst[:])

    prep(0)
    for gi in range(len(groups)):
        sb = P.record(lambda: compute(gi))
        sa = P.record(lambda: prep(gi + 1)) if gi + 1 < len(groups) else []
        P.play(sb, sa)
    P.release(m)


def phase_combine(P, io, sc, l, RT, out):
    m = P.mark()
    modL = ModSlice(P, sc, 0, 5120, 6144)
    modC = ModSlice(P, sc, 1, 5120, 6144)
    W = Ctx()
    ya = [P.sbuf("ya", [128, D], BF16) for _ in range(2)]
    yb = [P.sbuf("yb", [128, D], BF16) for _ in range(2)]
    xt = [P.sbuf("xc", [128, D], F32) for _ in range(2)]
    t1 = P.sbuf("t1", [128, D], F32)
    for bufs in (ya, yb):
        for b in bufs:
            P.memset(b[:], 0.0)
    last = l == DEPTH - 1
    for i in range(NT):
        if last and i >= 32:
            break
        mod = modC if i >= 32 else modL
        yat, ybt, x = ya[i % 2], yb[i % 2], xt[i % 2]
        P.dma("sp", x[:], sc.xres[i * 128:(i + 1) * 128, :])
        for a, yt in enumerate((yat, ybt)):
            idx = RT.slot[:, i, a:a + 1]
            P.dma("pool", yt[:], sc.YS[:, :], extra_reads=[idx],
                  indirect=lambda e, idx=idx, yt=yt: e.indirect_dma_start(
                      out=yt.t[:, :], out_offset=None, in_=sc.YS.t[:, :],
                      in_offset=bass.IndirectOffsetOnAxis(ap=idx.ap, axis=0),
                      bounds_check=P.breg, oob_is_err=False))
        P.ts(t1[:], yat[:], RT.gate[:, i, 0:1], None, ALU.mult)
        P.stt(t1[:], ybt[:], RT.gate[:, i, 1:2], t1[:], ALU.mult, ALU.add)
        P.tt(t1[:], t1[:], mod[:, 5120:6144], ALU.mult)
        P.tt(x[:], x[:], t1[:], ALU.add)
        if last:
            P.dma("sp", out[i * 128:(i + 1) * 128, :], x[:])
        else:
            P.dma("sp", sc.xres[i * 128:(i + 1) * 128, :], x[:])
    P.release(m)


def build_program(nlayers=DEPTH, dbg_x=False):
    nc = bass.Bass("TRN2", target_bir_lowering=False)
    P = Prog(nc)
    io = declare_io(P, None)
    sc = alloc_scratch(P)
    out = P.dram("out", [NLAT, D], F32, kind="ExternalOutput")
    dbg = P.dram("dbg_xres", [T, D], F32, kind="ExternalOutput") if dbg_x else None
    lbt = P.sbuf("lbt", [128, 4, 512], F32)
    RT = Ctx()
    RT.slot = P.sbuf("rt_slot", [128, NT, 2], I32)
    RT.gate = P.sbuf("rt_gate", [128, NT, 2], F32)
    P.breg = nc.gpsimd.to_reg(NSLOT - 1)
    phase_lb(P, io, lbt)
    for l in range(nlayers):
        phase_mod(P, io, sc, l)
        phase_proj(P, io, sc, l, lbt)
        phase_mla(P, io, sc)
        phase_na(P, io, sc, l)
        phase_gla(P, io, sc, l)
        phase_out(P, io, sc, l, RT)
        phase_experts(P, io, sc, l)
        phase_combine(P, io, sc, l if nlayers == DEPTH else -1, RT, out)
    if dbg_x:
        m = P.mark()
        xb = [P.sbuf("dbgx", [128, D], F32) for _ in range(2)]
        for i in range(NT):
            P.dma("sp", xb[i % 2][:], sc.xres[i * 128:(i + 1) * 128, :])
            P.dma("sp", dbg[i * 128:(i + 1) * 128, :], xb[i % 2][:])
        P.release(m)
    P.barrier()
    P.stats = (P.nins, P.nwaits, P.nsem)
    P.close()
    return nc


def make_in_maps(inputs):
    c = _consts()
    c.update(gla_consts())
    c.update(moe_consts())
    f = lambda a: np.ascontiguousarray(np.asarray(a, dtype=np.float32))
    gains = np.concatenate([inputs[k] for k in ["mla_g_cq", "mla_g_ckv", "mla_g_qn", "mla_g_qr", "mla_g_kn", "mla_g_kr",
                                                "na_g_q", "na_g_k", "ret_g_out", "hg_g_out"]], axis=1)
    shared = {
        "w_ada": f(inputs["w_ada"]), "b_ada": f(inputs["b_ada"]), "w_in": f(inputs["w_in"]), "w_out": f(inputs["w_out"]),
        "gains": f(gains), "mla_w_uq": f(inputs["mla_w_uq"]), "mla_w_ukv": f(inputs["mla_w_ukv"]),
        "ident": c["ident"], "rope": c["rope"], "hg_lb_raw": f(inputs["hg_lb_raw"]),
        "na_bias": na_bias_tables(np.asarray(inputs["na_rpb"], dtype=np.float32)),
        "gla_mask": c["gla_mask"], "gla_mm": c["gla_mm"], "gla_ci": c["gla_ci"], "ret_tab": c["ret_tab"], "ret_dec": c["ret_dec"],
        "moe_wr": f(np.concatenate([inputs["moe_w_rg"], inputs["moe_w_re"]], axis=2)),
        "moe_br": f(np.concatenate([inputs["moe_b_rg"], inputs["moe_b_re"]], axis=1)),
        "moe_U": c["moe_U"], "moe_eoff": c["moe_eoff"],
        "moe_w1": f(inputs["moe_w1"]), "moe_w3": f(inputs["moe_w3"]), "moe_w2": f(inputs["moe_w2"]),
    }
    maps = []
    for b in range(8):
        cv = np.stack([np.asarray(inputs["c"][b], np.float32), np.asarray(inputs["c_ctx"], np.float32)], axis=0)
        d = dict(shared)
        d["x"] = f(inputs["x"][b])
        d["ctx"] = f(inputs["ctx"][b])
        d["cvT"] = np.ascontiguousarray(cv.reshape(2, 8, 128).transpose(2, 1, 0))
        maps.append(d)
    return maps


def kernel(**inputs):
    nc = build_program()
    maps = make_in_maps(inputs)
    res = run_bass_kernel_spmd(nc, maps, core_ids=list(range(8)))
    return np.stack([np.asarray(r["out"], dtype=np.float32) for r in res.results], axis=0)
```
